# Optimizing a Trainium2 kernel written in Bass

```python
import jax, jax.numpy as jnp
from jax import lax
import numpy as np

D_MODEL = 1024
BATCH = 4
SEQ = 8192
DEPTH = 1

D_MIX = D_MODEL
D_ATTN = D_MIX // 2
N_HEADS = 8
HEAD_DIM = D_ATTN // N_HEADS
N_IDX_HEADS = 8
IDX_DIM = 64
TOPK_MAX = 256
Q_BLOCK = 128
ROPE_THETA = 10000.0
D_POOL = D_MIX - D_ATTN
POOL_WINDOWS = (2, 4, 8, 16)
N_POOL_GROUPS = 4
POOL_GROUP_DIM = D_POOL // N_POOL_GROUPS
SPLIT_POINTS = (D_ATTN,
                D_ATTN + HEAD_DIM,
                D_ATTN + 2 * HEAD_DIM,
                D_ATTN + 2 * HEAD_DIM + N_IDX_HEADS * IDX_DIM,
                D_ATTN + 2 * HEAD_DIM + N_IDX_HEADS * IDX_DIM + IDX_DIM,
                D_ATTN + 2 * HEAD_DIM + N_IDX_HEADS * IDX_DIM + IDX_DIM + N_IDX_HEADS)
D_IN = SPLIT_POINTS[-1] + D_POOL
N_GROUPS = 4
EXPERTS_PER_GROUP = 8
N_EXPERTS = N_GROUPS * EXPERTS_PER_GROUP
TOP_K = 2
D_EXPERT = 256
EPS = 1e-6

kernel_name = "hymba_dsa_pool_hmoe_adaln"


def rms_norm(x, g):
    xf = x.astype(jnp.float32)
    y = xf * lax.rsqrt(jnp.mean(xf * xf, axis=-1, keepdims=True) + EPS)
    return (y * g.astype(jnp.float32)).astype(x.dtype)


def rope_tables(positions, dim):
    inv_freq = ROPE_THETA ** (-jnp.arange(0, dim, 2, dtype=jnp.float32) / dim)
    ang = positions.astype(jnp.float32)[..., None] * inv_freq
    return jnp.cos(ang), jnp.sin(ang)


def apply_rope(x, cos, sin):
    x1, x2 = jnp.split(x.astype(jnp.float32), 2, axis=-1)
    return jnp.concatenate([x1 * cos - x2 * sin, x1 * sin + x2 * cos], axis=-1).astype(x.dtype)


def dsa_attention(q, kv, q_idx, k_idx, w_idx):
    B, S = q.shape[0], q.shape[1]
    topk = min(TOPK_MAX, S // 4)
    n_blocks = S // Q_BLOCK
    key_pos = jnp.arange(S)

    def block(i):
        start = i * Q_BLOCK
        qb = lax.dynamic_slice_in_dim(q, start, Q_BLOCK, axis=1)
        qib = lax.dynamic_slice_in_dim(q_idx, start, Q_BLOCK, axis=1)
        wb = lax.dynamic_slice_in_dim(w_idx, start, Q_BLOCK, axis=1)
        s_idx = jnp.einsum('bqhd,bsd->bqhs', qib, k_idx, preferred_element_type=jnp.float32)
        score = jnp.einsum('bqh,bqhs->bqs', wb, jax.nn.relu(s_idx))
        q_pos = start + jnp.arange(Q_BLOCK)
        causal = key_pos[None, :] <= q_pos[:, None]
        score = jnp.where(causal[None], score, -jnp.inf)
        top_val, top_ind = lax.top_k(score, topk)
        valid = top_val > -jnp.inf
        kv_sel = jax.vmap(lambda kvb, ib: kvb[ib])(kv, top_ind)
        k_sel, v_sel = jnp.split(kv_sel, 2, axis=-1)
        logits = jnp.einsum('bqhd,bqkd->bqhk', qb, k_sel,
                            preferred_element_type=jnp.float32) * (HEAD_DIM ** -0.5)
        logits = jnp.where(valid[:, :, None, :], logits, -jnp.inf)
        p = jax.nn.softmax(logits, axis=-1).astype(v_sel.dtype)
        return jnp.einsum('bqhk,bqkd->bqhd', p, v_sel)

    out = lax.map(block, jnp.arange(n_blocks))
    return jnp.moveaxis(out, 0, 1).reshape(B, S, N_HEADS * HEAD_DIM)


def pool_mixer(u, w_pool, pool_scale):
    B, S, _ = u.shape
    ug = u.reshape(B, S, N_POOL_GROUPS, POOL_GROUP_DIM)
    cs = jnp.cumsum(ug.astype(jnp.float32), axis=1)
    t = jnp.arange(S)
    outs = []
    for g, win in enumerate(POOL_WINDOWS):
        cg = cs[:, :, g]
        c_shift = jnp.pad(cg, ((0, 0), (win, 0), (0, 0)))[:, :S]
        cnt = jnp.minimum(t + 1, win).astype(jnp.float32)[None, :, None]
        outs.append((cg - c_shift) / cnt - ug[:, :, g].astype(jnp.float32))
    pooled = jnp.stack(outs, axis=2).astype(u.dtype)
    mixed = jnp.einsum('bsgc,gcd->bsgd', pooled, w_pool)
    return mixed.reshape(B, S, D_POOL) * pool_scale


def hierarchical_moe(h, w_rg, b_rg, w_re, b_re, w_gate, w_up, w_down):
    B, S, D = h.shape
    ht = h.reshape(B * S, D)
    g_logits = (ht @ w_rg + b_rg).astype(jnp.float32)
    p_group = jax.nn.softmax(g_logits, axis=-1)
    g_sel = jnp.argmax(g_logits, axis=-1)
    p_g = jnp.take_along_axis(p_group, g_sel[:, None], axis=-1)
    e_logits = (ht @ w_re + b_re).astype(jnp.float32).reshape(-1, N_GROUPS, EXPERTS_PER_GROUP)
    e_logits = jnp.take_along_axis(e_logits, g_sel[:, None, None], axis=1)[:, 0]
    p_e = jax.nn.softmax(e_logits, axis=-1)
    top_p, top_e = lax.top_k(p_e, TOP_K)
    top_p = top_p / jnp.sum(top_p, axis=-1, keepdims=True)
    flat = g_sel[:, None] * EXPERTS_PER_GROUP + top_e
    gates = jnp.sum(jax.nn.one_hot(flat, N_EXPERTS, dtype=jnp.float32)
                    * (p_g * top_p)[..., None], axis=1).astype(h.dtype)
    y = jnp.zeros_like(ht)
    for e in range(N_EXPERTS):
        a = jax.nn.silu(ht @ w_gate[e]) * (ht @ w_up[e])
        y = y + gates[:, e:e + 1] * (a @ w_down[e])
    return y.reshape(B, S, D)


def setup_inputs(seed: int = 0) -> dict:
    key = jax.random.key(seed)
    ks = jax.random.split(key, 22)
    f32 = jnp.float32
    nrm = lambda k, shape, s: jax.random.normal(k, shape, f32) * s
    x = jax.random.normal(ks[0], (BATCH, SEQ, D_MODEL), f32)
    c = jax.random.normal(ks[1], (BATCH, D_MODEL), f32)
    offset = jax.random.randint(ks[2], (BATCH, 1), 0, 4096, dtype=jnp.int32)
    positions = (offset + jnp.arange(SEQ, dtype=jnp.int32)[None, :]).astype(jnp.int32)
    return {
        "x": x,
        "c": c,
        "positions": positions,
        "w_ada": nrm(ks[3], (DEPTH, D_MODEL, 6 * D_MODEL), 0.5 * D_MODEL ** -0.5),
        "b_ada": nrm(ks[4], (DEPTH, 6 * D_MODEL), 0.02),
        "g_norm_mix": 1.0 + nrm(ks[5], (DEPTH, D_MODEL), 0.1),
        "g_norm_ffn": 1.0 + nrm(ks[6], (DEPTH, D_MODEL), 0.1),
        "w_in": nrm(ks[7], (DEPTH, D_MODEL, D_IN), D_MODEL ** -0.5),
        "g_q": 1.0 + nrm(ks[8], (DEPTH, HEAD_DIM), 0.1),
        "g_k": 1.0 + nrm(ks[9], (DEPTH, HEAD_DIM), 0.1),
        "g_kidx": 1.0 + nrm(ks[10], (DEPTH, IDX_DIM), 0.1),
        "w_pool": nrm(ks[11], (DEPTH, N_POOL_GROUPS, POOL_GROUP_DIM, POOL_GROUP_DIM), POOL_GROUP_DIM ** -0.5),
        "pool_scale": 0.5 + nrm(ks[12], (DEPTH, D_POOL), 0.1),
        "w_out": nrm(ks[13], (DEPTH, D_MIX, D_MODEL), D_MIX ** -0.5),
        "w_router_group": nrm(ks[14], (DEPTH, D_MODEL, N_GROUPS), D_MODEL ** -0.5),
        "b_router_group": nrm(ks[15], (DEPTH, N_GROUPS), 0.01),
        "w_router_expert": nrm(ks[16], (DEPTH, D_MODEL, N_EXPERTS), D_MODEL ** -0.5),
        "b_router_expert": nrm(ks[17], (DEPTH, N_EXPERTS), 0.01),
        "w_gate": nrm(ks[18], (DEPTH, N_EXPERTS, D_MODEL, D_EXPERT), D_MODEL ** -0.5),
        "w_up": nrm(ks[19], (DEPTH, N_EXPERTS, D_MODEL, D_EXPERT), D_MODEL ** -0.5),
        "w_down": nrm(ks[20], (DEPTH, N_EXPERTS, D_EXPERT, D_MODEL), D_EXPERT ** -0.5),
    }


def reference(x, c, positions, w_ada, b_ada, g_norm_mix, g_norm_ffn, w_in, g_q, g_k, g_kidx,
              w_pool, pool_scale, w_out, w_router_group, b_router_group, w_router_expert,
              b_router_expert, w_gate, w_up, w_down):
    B, S, D = x.shape
    cos, sin = rope_tables(positions, HEAD_DIM)
    cos_h, sin_h = cos[:, :, None, :], sin[:, :, None, :]
    c_act = jax.nn.silu(c)
    for l in range(DEPTH):
        mod = c_act @ w_ada[l] + b_ada[l]
        shift1, scale1, gate1, shift2, scale2, gate2 = [m[:, None, :] for m in jnp.split(mod, 6, axis=-1)]

        h = rms_norm(x, g_norm_mix[l]) * (1.0 + scale1) + shift1
        proj = h @ w_in[l]
        q, k, v, q_idx, k_idx, w_idx, u = jnp.split(proj, SPLIT_POINTS, axis=-1)
        q = apply_rope(rms_norm(q.reshape(B, S, N_HEADS, HEAD_DIM), g_q[l]), cos_h, sin_h)
        k = apply_rope(rms_norm(k, g_k[l]), cos, sin)
        kv = jnp.concatenate([k, v], axis=-1)
        q_idx = apply_rope(q_idx.reshape(B, S, N_IDX_HEADS, IDX_DIM), cos_h, sin_h)
        k_idx = apply_rope(rms_norm(k_idx, g_kidx[l]), cos, sin)
        w_idx = w_idx.astype(jnp.float32) * (N_IDX_HEADS ** -0.5 * IDX_DIM ** -0.5)
        attn_out = dsa_attention(q, kv, q_idx, k_idx, w_idx)
        pool_out = pool_mixer(u, w_pool[l], pool_scale[l])
        mix = jnp.concatenate([attn_out, pool_out], axis=-1) @ w_out[l]
        x = x + gate1 * mix

        h2 = rms_norm(x, g_norm_ffn[l]) * (1.0 + scale2) + shift2
        x = x + gate2 * hierarchical_moe(h2, w_router_group[l], b_router_group[l],
                                         w_router_expert[l], b_router_expert[l],
                                         w_gate[l], w_up[l], w_down[l])
    return x
```

```python
import numpy as np
from contextlib import ExitStack
import concourse.bass as bass
import concourse.mybir as mybir
from concourse.bass_utils import run_bass_kernel_spmd

F32 = mybir.dt.float32
BF16 = mybir.dt.bfloat16
I32 = mybir.dt.int32
U8 = mybir.dt.uint8
AF = mybir.ActivationFunctionType
ALU = mybir.AluOpType
AX = mybir.AxisListType

D = 1024
NB = 32
NSLOT = 64
GB = 4
NIT = 22
DVE_FRAC = 0.50
NE = 32
DE = 256
EPS = 1e-6
NEG = -3.0e38
MNEG = -30000.0
ARENA_KB = 206

ENGS = ("pe", "act", "dve", "pool", "sp")
NO_SELF = ("pe", "sp")

DEBUG = {}
STOP_AT = [99]


class Buf:
    __slots__ = ("name", "w", "rs", "excl")

    def __init__(self, name, excl=False):
        self.name = name
        self.w = None
        self.rs = {}
        self.excl = excl


class Ctr:
    __slots__ = ("sem", "n", "name")

    def __init__(self, sem, name):
        self.sem = sem
        self.n = 0
        self.name = name


class T:
    def __init__(self, ap, bufs):
        self.ap = ap
        self.bufs = bufs if isinstance(bufs, (list, tuple)) else [bufs]


def _bufs(xs):
    out = []
    for x in xs:
        if x is None:
            continue
        if isinstance(x, Buf):
            out.append(x)
        elif isinstance(x, T):
            out.extend(x.bufs)
        else:
            out.extend(_bufs(x))
    return out


class Sched:
    def __init__(self, nc, es):
        self.nc = nc
        self.es = es
        self.q = {e: [] for e in ENGS}
        self.nsem = 0
        self.ectr = {e: self.ctr("eng_" + e) for e in ENGS}
        self.waited = {e: {} for e in ENGS}
        self.dma_ctrs = []
        self.stopped = False
        self.deferred = None

    def stage(self, n):
        if STOP_AT[0] <= n:
            self.stopped = True

    def ctr(self, name):
        self.nsem += 1
        return Ctr(self.es.enter_context(self.nc.semaphore("s_" + name)), name)

    def dctr(self, name):
        c = self.ctr(name)
        self.dma_ctrs.append(c)
        return c

    def _collect(self, eng, rb, wb, extra=()):
        need = {}
        me = self.ectr[eng]
        wd = self.waited[eng]

        def add(c, v):
            if c is me and eng in NO_SELF:
                return
            if wd.get(c, 0) >= v:
                return
            if need.get(c, 0) < v:
                need[c] = v

        for b in rb:
            if b.w is not None:
                add(*b.w)
        for b in wb:
            if b.w is not None:
                add(*b.w)
            for c, v in b.rs.items():
                add(c, v)
        for c, v in extra:
            add(c, v)
        for c, v in need.items():
            wd[c] = v
        return [(c.sem, v) for c, v in need.items()]

    def defer_begin(self):
        self.deferred = []

    def defer_end(self):
        d = self.deferred
        self.deferred = None
        return d

    def emit_some(self, pend, n):
        while pend and n > 0:
            kind, args = pend.pop(0)
            (self.op if kind == "op" else self.dma)(*args)
            n -= 1

    def op(self, eng, fn, reads=(), writes=()):
        if self.stopped:
            return
        if self.deferred is not None:
            self.deferred.append(("op", (eng, fn, reads, writes)))
            return
        rb = _bufs(reads)
        wb = _bufs(writes)
        if any(b.excl for b in rb):
            wb = wb + [b for b in rb if b.excl]
            rb = [b for b in rb if not b.excl]
        waits = self._collect(eng, rb, wb)
        c = self.ectr[eng]
        c.n += 1
        self.q[eng].append((waits, fn, c.sem, 1))
        for b in rb:
            if b.rs.get(c, 0) < c.n:
                b.rs[c] = c.n
        for b in wb:
            b.w = (c, c.n)
            b.rs = {}

    def dma(self, eng, fn, reads, writes, ctr):
        if self.stopped:
            return
        if self.deferred is not None:
            self.deferred.append(("dma", (eng, fn, reads, writes, ctr)))
            return
        rb = _bufs(reads)
        wb = _bufs(writes)
        waits = self._collect(eng, rb, wb)
        ctr.n += 16
        self.q[eng].append((waits, fn, ctr.sem, 16))
        for b in rb:
            if b.rs.get(ctr, 0) < ctr.n:
                b.rs[ctr] = ctr.n
        for b in wb:
            b.w = (ctr, ctr.n)
            b.rs = {}

    def barrier(self):
        if self.stopped:
            return
        snap = [(self.ectr[e], self.ectr[e].n) for e in ENGS if self.ectr[e].n > 0]
        snap += [(c, c.n) for c in self.dma_ctrs if c.n > 0]
        for e in ENGS:
            waits = self._collect(e, [], [], extra=snap)
            if waits:
                self.q[e].append((waits, None, None, 0))

    def final_wait(self, eng="sp"):
        self.stopped = False
        snap = [(self.ectr[e], self.ectr[e].n) for e in ENGS if self.ectr[e].n > 0 and e != eng]
        snap += [(c, c.n) for c in self.dma_ctrs if c.n > 0]
        waits = self._collect(eng, [], [], extra=snap)
        if waits:
            self.q[eng].append((waits, None, None, 0))

    def replay(self, eng, e):
        for waits, fn, sem, inc in self.q[eng]:
            for s, v in waits:
                e.wait_ge(s, v)
            if fn is not None:
                ins = fn(e)
                ins.then_inc(sem, inc)


class Arena:
    def __init__(self, ap_u8, nbytes):
        self.ap = ap_u8
        self.nbytes = nbytes
        self.off = 0

    def mark(self):
        return self.off

    def reset(self, m):
        self.off = m

    def tile(self, name, free_shape, dt, nbufs=None, parts=128):
        esz = {F32: 4, BF16: 2, I32: 4, U8: 1}[dt]
        n = 1
        for s in free_shape:
            n *= s
        nb = n * esz
        self.off = (self.off + 63) // 64 * 64
        assert self.off + nb <= self.nbytes, f"arena overflow at {name}: {self.off + nb} > {self.nbytes}"
        ap = self.ap[:, self.off:self.off + nb]
        self.off += nb
        if dt != U8:
            ap = ap.bitcast(dt)
        if len(free_shape) == 2:
            ap = ap.rearrange("p (a b) -> p a b", a=free_shape[0])
        elif len(free_shape) == 3:
            ap = ap.rearrange("p (a b c) -> p a b c", a=free_shape[0], b=free_shape[1])
        if nbufs is None:
            bufs = [Buf(name)]
        else:
            bufs = [Buf(f"{name}{i}") for i in range(nbufs)]
        return T(ap, bufs)


def build_program(dbg=None):
    dbg = dbg or {}
    nc = bass.Bass("TRN2", target_bir_lowering=False, dynamic_dma_scratch_size=1024)
    dram = {}

    def din(name, shape, dt=F32):
        dram[name] = nc.dram_tensor(name, list(shape), dt, kind="ExternalInput").ap()
        return dram[name]

    x_own = din("x_own", [NB * 128, D])
    x_oth = din("x_oth", [NB * 128, D])
    pos_in = din("pos", [128, NSLOT], I32)
    colp = din("colp", [128, 72])
    rowp = din("rowp", [1, 832])
    bgate = din("bgate", [1, 2048])
    meta = din("meta", [128, 2])
    rc0_in = din("rc0", [128, 4 * 128])
    w_ada = din("w_ada", [D, 6 * D])
    w_in = din("w_in", [D, 1736])
    w_out = din("w_out", [D, D])
    w_pool = din("w_pool", [128, 4 * 128])
    w_r = din("w_r", [D, 36])
    w_gate = din("w_gate", [NE, D, DE])
    w_up = din("w_up", [NE, D, DE])
    w_down = din("w_down", [NE, DE, D])
    out_d = nc.dram_tensor("out", [NB * 128, D], F32, kind="ExternalOutput").ap()
    wgu_s = nc.dram_tensor("wgu_s", [NE, 128, 8 * 512], BF16, kind="Internal").ap()
    wd_s = nc.dram_tensor("wd_s", [NE, 128, 2 * D], BF16, kind="Internal").ap()
    dbg_d = {}
    for k, (shape, dt) in dbg.items():
        dbg_d[k] = nc.dram_tensor("dbg_" + k, list(shape), dt, kind="ExternalOutput").ap()

    with ExitStack() as es:
        arena_t = es.enter_context(nc.sbuf_tensor("arena", [128, ARENA_KB * 1024], U8))
        ps_t = es.enter_context(nc.psum_tensor("ps", [128, 8, 512], F32))
        S = Sched(nc, es)
        A = Arena(arena_t[:, :], ARENA_KB * 1024)
        pbank = [Buf(f"bank{i}", excl=True) for i in range(8)]

        def PS(b0, nb=1):
            ap = ps_t[:, b0:b0 + nb, :].rearrange("p a b -> p (a b)") if nb > 1 else ps_t[:, b0, :]
            return T(ap, pbank[b0:b0 + nb])

        dbg_ctr = S.dctr("dbg") if dbg else None

        def dump(name, t, ap=None):
            if name not in dbg_d:
                return
            src = ap if ap is not None else t.ap
            S.dma("sp", lambda e, o=dbg_d[name], i=src: e.dma_start(out=o, in_=i), [t], [], dbg_ctr)

        ident_f = A.tile("ident_f", [128], F32)
        ident_b = A.tile("ident_b", [128], BF16)
        irep = A.tile("irep", [4, 128], BF16)
        tri = A.tile("tri", [128], F32)
        ab = A.tile("ab", [4, 8], F32)
        G1 = A.tile("G1", [D], F32)
        G2 = A.tile("G2", [D], F32)
        rowb = A.tile("rowb", [832], F32)
        gq8 = A.tile("gq8", [64], F32)
        metat = A.tile("metat", [2], F32)
        rc0 = A.tile("rc0", [4, 128], F32)
        win = A.tile("win", [8, 1736], BF16)
        wout = A.tile("wout", [8, D], BF16)
        wpool = A.tile("wpool", [4, 128], BF16)
        wr = A.tile("wr", [8, 36], F32)
        kk = A.tile("kk", [NSLOT * 128], BF16, nbufs=NSLOT)
        Vt = A.tile("V", [NSLOT, 66], BF16, nbufs=NSLOT)
        sint = A.tile("sin", [NSLOT, 32], F32)
        cost = A.tile("cos", [NSLOT, 32], F32)
        acc = [A.tile(f"acc{g}", [D], F32) for g in range(GB)]
        h2T = A.tile("h2T", [8, GB * 128], BF16, nbufs=GB)
        gates = [A.tile(f"gates{g}", [NE], F32) for g in range(GB)]
        tailt = A.tile("tailt", [4, 16], F32)
        pmark = A.mark()
        cols = A.tile("cols", [72], F32)
        modc = A.tile("modc", [48], F32)
        sc2 = A.tile("sc2", [8, 2], F32)

        gk_bc = T(rowb.ap[:, 64:192], rowb.bufs)
        pscale_bc = T(rowb.ap[:, 192:704], rowb.bufs)
        brt_bc = T(rowb.ap[:, 704:740], rowb.bufs)
        invf_bc = T(rowb.ap[:, 740:772], rowb.bufs)
        pow2_bc = T(rowb.ap[:, 772:796], rowb.bufs)

        setup_ctr = S.dctr("setup")

        def ld(t, src, ap=None, ctr=None):
            dst = ap if ap is not None else t.ap
            S.dma("sp", lambda e, o=dst, i=src: e.dma_start(out=o, in_=i), [], [t], ctr or setup_ctr)

        ld(cols, colp)
        ld(rowb, rowp.partition_broadcast(128).rearrange("p a b -> p (a b)"))
        ld(metat, meta)
        ld(rc0, rc0_in.rearrange("p (a b) -> p a b", a=4))
        ld(wr, w_r.rearrange("(kc p) n -> p kc n", p=128))
        st_pos = A.tile("st_pos", [NSLOT], I32)
        ld(st_pos, pos_in)
        st_bg = A.tile("st_bg", [2048], F32, parts=1)
        ld(st_bg, bgate, ap=st_bg.ap[0:1, :])
        fin = (setup_ctr, setup_ctr.n)
        for t in (cols, rowb, metat, rc0, wr, st_pos, st_bg):
            t.bufs[0].w = fin

        io_i = A.tile("io_i", [128], I32)
        io_f = A.tile("io_f", [128], F32)
        S.op("pool", lambda e: e.iota(io_i.ap, pattern=[[1, 128]], base=0, channel_multiplier=-1), [], [io_i])
        S.op("dve", lambda e: e.tensor_copy(out=io_f.ap, in_=io_i.ap), [io_i], [io_f])
        S.op("dve", lambda e: e.tensor_scalar(out=ident_f.ap, in0=io_f.ap, scalar1=0.0, scalar2=None, op0=ALU.is_equal), [io_f], [ident_f])
        S.op("dve", lambda e: e.tensor_copy(out=ident_b.ap, in_=ident_f.ap), [ident_f], [ident_b])
        S.op("dve", lambda e: e.tensor_copy(out=irep.ap, in_=ident_f.ap.unsqueeze(1).to_broadcast([128, 4, 128])), [ident_f], [irep])
        S.op("dve", lambda e: e.tensor_scalar(out=tri.ap, in0=io_f.ap, scalar1=0.0, scalar2=NEG, op0=ALU.is_gt, op1=ALU.mult), [io_f], [tri])
        ones1 = A.tile("ones1", [128], F32, parts=1)
        S.op("dve", lambda e: e.memset(ones1.ap[0:1, :], 1.0), [], [ones1])
        S.op("dve", lambda e: e.memset(Vt.ap[:, :, 64:65], 1.0), [], [Vt])
        S.op("dve", lambda e: e.memset(Vt.ap[:, :, 65:66], 0.0), [], [Vt])
        S.op("dve", lambda e: e.tensor_scalar(out=gq8.ap, in0=rowb.ap[:, 0:64], scalar1=0.125, scalar2=None, op0=ALU.mult), [rowb], [gq8])

        S.op("act", lambda e: e.activation(out=sc2.ap[:, :, 0], in_=cols.ap[:, 0:8], func=AF.Silu), [cols], [sc2])
        S.op("act", lambda e: e.activation(out=sc2.ap[:, :, 1], in_=cols.ap[:, 0:8], func=AF.Silu), [cols], [sc2])
        scb = A.tile("scb", [8, 128], F32)
        S.op("dve", lambda e: e.tensor_copy(out=scb.ap, in_=sc2.ap[:, :, 0:1].to_broadcast([128, 8, 128])), [sc2], [scb])

        stg = [A.tile(f"stg{i}", [8, 512], F32) for i in range(2)]
        stg_ctr = [S.dctr(f"stg{i}") for i in range(2)]
        wada_v = w_ada.rearrange("(kc p) n -> p kc n", p=128)
        mod_ps = PS(0)
        mod_v = mod_ps.ap[:, 0:96].rearrange("p (a b) -> p a b", b=2)
        first_mod = True
        for i in range(12):
            st = stg[i % 2]
            S.dma("sp", lambda e, o=st.ap, s=wada_v[:, :, i * 512:(i + 1) * 512]: e.dma_start(out=o, in_=s), [], [st], stg_ctr[i % 2])
            for sub in range(4):
                col = i * 4 + sub
                for kc in range(8):
                    S.op("pe", lambda e, o=mod_v[:, col, :], l=st.ap[:, kc, sub * 128:(sub + 1) * 128], r=sc2.ap[:, kc, :], f=first_mod, kc=kc:
                         e.matmul(o, l, r, start=f, stop=(kc == 7), skip_group_check=True), [st, sc2], [mod_ps])
                    first_mod = False
            if i in (4, 5, 10, 11):
                gt = G1 if i < 6 else G2
                half = i % 2
                boff = (0 if i < 6 else 1024) + half * 512
                gps = PS(1 + (i % 2))
                for kc in range(8):
                    S.op("pe", lambda e, o=gps.ap, l=scb.ap[:, kc, :], r=st.ap[:, kc, :], kc=kc:
                         e.matmul(o, l, r, start=(kc == 0), stop=False), [st, scb], [gps])
                S.op("pe", lambda e, o=gps.ap, l=ones1.ap[0:1, :], r=st_bg.ap[0:1, boff:boff + 512]:
                     e.matmul(o, l, r, start=False, stop=True), [ones1, st_bg], [gps])
                S.op("act", lambda e, o=gt.ap[:, half * 512:(half + 1) * 512], i_=gps.ap: e.copy(out=o, in_=i_), [gps], [gt])
        S.op("dve", lambda e: e.tensor_tensor(out=modc.ap, in0=mod_v[:, :, 0], in1=cols.ap[:, 24:72], op=ALU.add), [mod_ps, cols], [modc])
        S.op("dve", lambda e: e.scalar_tensor_tensor(out=ab.ap[:, 0, :], in0=modc.ap[:, 8:16], scalar=1.0, in1=cols.ap[:, 8:16], op0=ALU.add, op1=ALU.mult), [modc, cols], [ab])
        S.op("dve", lambda e: e.tensor_copy(out=ab.ap[:, 1, :], in_=modc.ap[:, 0:8]), [modc], [ab])
        S.op("dve", lambda e: e.scalar_tensor_tensor(out=ab.ap[:, 2, :], in0=modc.ap[:, 32:40], scalar=1.0, in1=cols.ap[:, 16:24], op0=ALU.add, op1=ALU.mult), [modc, cols], [ab])
        S.op("dve", lambda e: e.tensor_copy(out=ab.ap[:, 3, :], in_=modc.ap[:, 24:32]), [modc], [ab])
        dump("ab", ab)
        dump("G1", G1)

        win_v = w_in.rearrange("(kc p) n -> p kc n", p=128)
        k = 0
        for (c0, c1) in ((0, 512), (512, 1024), (1024, 1536), (1536, 1736)):
            st = stg[k % 2]
            S.dma("sp", lambda e, o=st.ap[:, :, 0:c1 - c0], s=win_v[:, :, c0:c1]: e.dma_start(out=o, in_=s), [], [st], stg_ctr[k % 2])
            S.op("pool", lambda e, o=win.ap[:, :, c0:c1], i_=st.ap[:, :, 0:c1 - c0]: e.tensor_copy(out=o, in_=i_), [st], [win])
            k += 1
        wout_v = w_out.rearrange("(kc p) n -> p kc n", p=128)
        for (c0, c1) in ((0, 512), (512, 1024)):
            st = stg[k % 2]
            S.dma("sp", lambda e, o=st.ap, s=wout_v[:, :, c0:c1]: e.dma_start(out=o, in_=s), [], [st], stg_ctr[k % 2])
            S.op("pool", lambda e, o=wout.ap[:, :, c0:c1], i_=st.ap: e.tensor_copy(out=o, in_=i_), [st], [wout])
            k += 1
        st = stg[k % 2]
        S.dma("sp", lambda e, o=st.ap[:, 0, :], s=w_pool: e.dma_start(out=o, in_=s), [], [st], stg_ctr[k % 2])
        S.op("pool", lambda e, o=wpool.ap, i_=st.ap[:, 0, :].rearrange("p (a b) -> p a b", a=4): e.tensor_copy(out=o, in_=i_), [st], [wpool])
        k += 1

        TWO_PI = 6.283185307179586
        C1 = 6.28125
        C2 = TWO_PI - C1
        MAGIC = 12582912.0
        PI_LO = 3.1415925
        posf = A.tile("posf", [NSLOT], F32)
        ang = A.tile("ang", [NSLOT, 32], F32)
        kr = A.tile("kr", [NSLOT, 32], F32)
        r1 = A.tile("r1", [NSLOT, 32], F32)
        S.op("dve", lambda e: e.tensor_copy(out=posf.ap, in_=st_pos.ap), [st_pos], [posf])
        S.op("dve", lambda e: e.tensor_tensor(out=ang.ap, in0=posf.ap.unsqueeze(2).to_broadcast([128, NSLOT, 32]),
                                              in1=invf_bc.ap.unsqueeze(1).to_broadcast([128, NSLOT, 32]), op=ALU.mult), [posf, rowb], [ang])
        for which, tab in ((0, sint), (1, cost)):
            off = 0.0 if which == 0 else 0.25
            S.op("dve", lambda e, off=off: e.tensor_scalar(out=kr.ap, in0=ang.ap, scalar1=1.0 / TWO_PI, scalar2=off, op0=ALU.mult, op1=ALU.add), [ang], [kr])
            S.op("dve", lambda e: e.tensor_scalar(out=kr.ap, in0=kr.ap, scalar1=MAGIC, scalar2=None, op0=ALU.add), [kr], [kr])
            S.op("dve", lambda e: e.tensor_scalar(out=kr.ap, in0=kr.ap, scalar1=MAGIC, scalar2=None, op0=ALU.subtract), [kr], [kr])
            S.op("dve", lambda e: e.scalar_tensor_tensor(out=r1.ap, in0=kr.ap, scalar=-C1, in1=ang.ap, op0=ALU.mult, op1=ALU.add), [kr, ang], [r1])
            S.op("dve", lambda e: e.scalar_tensor_tensor(out=r1.ap, in0=kr.ap, scalar=-C2, in1=r1.ap, op0=ALU.mult, op1=ALU.add), [kr, r1], [r1])
            if which == 1:
                S.op("dve", lambda e: e.tensor_scalar(out=r1.ap, in0=r1.ap, scalar1=TWO_PI / 4, scalar2=None, op0=ALU.add), [r1], [r1])
            S.op("dve", lambda e: e.tensor_scalar(out=r1.ap, in0=r1.ap, scalar1=-PI_LO, scalar2=PI_LO, op0=ALU.max, op1=ALU.min), [r1], [r1])
            S.op("act", lambda e, tab=tab: e.activation(out=tab.ap, in_=r1.ap, func=AF.Sin), [r1], [tab])
        dump("sin", sint)
        dump("cos", cost)
        S.stage(1)

        S.barrier()
        A.reset(pmark)

        amark = A.mark()
        It = A.tile("I", [NSLOT * 128], F32, nbufs=8)
        relu = [A.tile(f"relu{i}", [128, 8], F32) for i in range(3)]
        jk = A.tile("jk", [2], F32)
        xn = A.tile("xn", [D], F32)
        hT = A.tile("hT", [8, 128], BF16)
        QQ = A.tile("QQ", [8, 128], BF16)
        QQT = A.tile("QQT", [8, 128], BF16)
        qn = A.tile("qn", [8, 64], F32)
        rt = [A.tile(f"rt{i}", [8, 32], F32) for i in range(2)]
        KK = A.tile("KK", [128], BF16)
        ext = A.tile("ext", [4, 144], F32)
        pA = A.tile("pA", [4, 144], F32)
        pB = A.tile("pB", [4, 144], F32)
        pooledT = A.tile("pooledT", [4, 128], BF16)
        mixin = A.tile("mixin", [D], BF16)
        Mb = [A.tile(f"Mb{i}", [512], BF16) for i in range(2)]
        PT = [A.tile(f"PT{i}", [D], BF16) for i in range(2)]
        h2Tf = A.tile("h2Tf", [8, 128], F32)
        sm = A.tile("sm", [64], F32)
        steps = A.tile("steps", [NIT + 2], F32)
        nsteps = A.tile("nsteps", [NIT + 2], F32)
        tst = A.tile("tst", [2], F32)
        sm2 = A.tile("sm2", [2], F32)
        smo = A.tile("smo", [18], F32)
        wsc = A.tile("wsc", [8], F32)
        lg = A.tile("lg", [36], F32)
        rsm = A.tile("rsm", [96], F32)
        x_ctr = [S.dctr(f"xacc{g}") for g in range(GB)]
        xo_ctr = S.dctr("xo")
        out_ctr = [S.dctr(f"out{g}") for g in range(GB)]
        PT = PT + [T(r.ap.rearrange("p k h -> p (k h)").bitcast(BF16)[:, 0:D], r.bufs) for r in relu[0:2]]
        QQTz = T(relu[2].ap.rearrange("p k h -> p (k h)").bitcast(BF16)[:, 0:D], relu[2].bufs)
        att_end = A.mark()

        smk = [0]

        def col(n=1):
            o = smk[0]
            smk[0] += n
            assert smk[0] <= 64
            return sm.ap[:, o:o + n]

        c_ss = col()
        c_rstd = col()
        c_ssh = col(8)
        c_rh = col(8)
        CS_MAIN = (c_ss, c_rstd, c_ssh, c_rh, sm)
        CS_OTH = (smo.ap[:, 0:1], smo.ap[:, 1:2], smo.ap[:, 2:10], smo.ap[:, 10:18], smo)
        c_B = col()
        c_lo = col()
        c_test = col()
        c_cnt = col()
        c_g = col()
        c_rden = col(8)

        def rms_to_hT(xt, a_idx, dst_bf, dst_f32=None, cs=None):
            c_ss, c_rstd, _, _, sm = cs or CS_MAIN
            if xt is xn:
                S.op("act", lambda e: e.activation(out=jk.ap[:, 0:1].to_broadcast([128, D]), in_=xt.ap, func=AF.Square, accum_out=c_ss), [xt], [jk, sm])
            else:
                S.op("act", lambda e: e.activation(out=xn.ap, in_=xt.ap, func=AF.Square, accum_out=c_ss), [xt], [xn, sm])
            S.op("dve", lambda e: e.tensor_scalar(out=c_rstd, in0=c_ss, scalar1=1.0 / D, scalar2=EPS, op0=ALU.mult, op1=ALU.add), [sm], [sm])
            S.op("act", lambda e: e.activation(out=c_rstd, in_=c_rstd, func=AF.Sqrt), [sm], [sm])
            S.op("dve", lambda e: e.reciprocal(out=c_rstd, in_=c_rstd), [sm], [sm])
            S.op("act", lambda e: e.activation(out=xn.ap, in_=xt.ap, func=AF.Identity, scale=c_rstd), [xt, sm], [xn])
            tp = PS(6, 2)
            tpv = tp.ap.rearrange("p (a b) -> p a b", a=8)
            for c in range(8):
                S.op("pe", lambda e, c=c: e.transpose(tpv[:, c, :], xn.ap[:, c * 128:(c + 1) * 128], ident_f.ap), [xn, ident_f], [tp])
            for c in range(8):
                S.op("act", lambda e, c=c: e.activation(out=dst_bf[0][:, c, :], in_=tpv[:, c, :], func=AF.Identity,
                                                        scale=ab.ap[:, a_idx, c:c + 1], bias=ab.ap[:, a_idx + 1, c:c + 1]), [tp, ab], [dst_bf[1]])
                if dst_f32 is not None:
                    S.op("dve", lambda e, c=c: e.tensor_scalar(out=dst_f32.ap[:, c, :], in0=tpv[:, c, :], scalar1=ab.ap[:, a_idx, c:c + 1],
                                                               scalar2=ab.ap[:, a_idx + 1, c:c + 1], op0=ALU.mult, op1=ALU.add), [tp, ab], [dst_f32])

        def head_norm(src_ps, nh, gbc, dst, cs=None):
            _, _, c_ssh, c_rh, sm = cs or CS_MAIN
            sv = src_ps[0].rearrange("p (h d) -> p h d", h=nh)
            S.op("act", lambda e: e.activation(out=dst.ap[:, 0:nh, :], in_=sv, func=AF.Square), [src_ps[1]], [dst])
            S.op("dve", lambda e: e.tensor_reduce(out=c_ssh[:, 0:nh], in_=dst.ap[:, 0:nh, :], axis=AX.X, op=ALU.add), [dst], [sm])
            S.op("dve", lambda e: e.tensor_scalar(out=c_rh[:, 0:nh], in0=c_ssh[:, 0:nh], scalar1=1.0 / 64, scalar2=EPS, op0=ALU.mult, op1=ALU.add), [sm], [sm])
            S.op("act", lambda e: e.activation(out=c_rh[:, 0:nh], in_=c_rh[:, 0:nh], func=AF.Sqrt), [sm], [sm])
            S.op("dve", lambda e: e.reciprocal(out=c_rh[:, 0:nh], in_=c_rh[:, 0:nh]), [sm], [sm])
            for h in range(nh):
                S.op("dve", lambda e, h=h: e.scalar_tensor_tensor(out=dst.ap[:, h, :], in0=sv[:, h, :], scalar=c_rh[:, h:h + 1],
                                                                  in1=gbc[:, h * 64 % gbc.shape[1]:h * 64 % gbc.shape[1] + 64], op0=ALU.mult, op1=ALU.mult),
                     [src_ps[1], sm, rowb, gq8], [dst])

        def rope(src_ap, src_dep, nh, slot, dst_ap, dst_dep):
            s4 = src_ap.rearrange("p h (two d) -> p h two d", two=2)
            d4 = dst_ap.rearrange("p h (two d) -> p h two d", two=2)
            cb = cost.ap[:, slot, :].unsqueeze(1).to_broadcast([128, nh, 32])
            sb = sint.ap[:, slot, :].unsqueeze(1).to_broadcast([128, nh, 32])
            t0 = rt[0].ap[:, 0:nh, :]
            t1 = rt[1].ap[:, 0:nh, :]
            x1 = s4[:, :, 0, :]
            x2 = s4[:, :, 1, :]
            S.op("dve", lambda e: e.tensor_tensor(out=t0, in0=x1, in1=cb, op=ALU.mult), [src_dep, cost], [rt[0]])
            S.op("dve", lambda e: e.tensor_tensor(out=t1, in0=x2, in1=sb, op=ALU.mult), [src_dep, sint], [rt[1]])
            S.op("dve", lambda e: e.tensor_tensor(out=d4[:, :, 0, :], in0=t0, in1=t1, op=ALU.subtract), [rt[0], rt[1]], [dst_dep])
            S.op("dve", lambda e: e.tensor_tensor(out=t0, in0=x1, in1=sb, op=ALU.mult), [src_dep, sint], [rt[0]])
            S.op("dve", lambda e: e.tensor_tensor(out=t1, in0=x2, in1=cb, op=ALU.mult), [src_dep, cost], [rt[1]])
            S.op("dve", lambda e: e.tensor_tensor(out=d4[:, :, 1, :], in0=t0, in1=t1, op=ALU.add), [rt[0], rt[1]], [dst_dep])

        def kside(slot, kv_ps, cs=None):
            kkb = kk.bufs[slot]
            vb = Vt.bufs[slot]
            head_norm((kv_ps.ap[:, 0:128], kv_ps), 2, gk_bc.ap, qn, cs=cs)
            rope(qn.ap[:, 0:2, :], qn, 2, slot, KK.ap.rearrange("p (h d) -> p h d", h=2), KK)
            S.op("act", lambda e: e.copy(out=Vt.ap[:, slot, 0:64], in_=kv_ps.ap[:, 128:192]), [kv_ps], [vb])
            kt = PS(3)
            ktv = kt.ap.bitcast(BF16)[:, 0:128]
            S.op("pe", lambda e: e.transpose(ktv, KK.ap, ident_b.ap), [KK, ident_b], [kt])
            S.op("act", lambda e: e.copy(out=kk.ap[:, slot * 128:(slot + 1) * 128], in_=ktv), [kt], [kkb])

        def other_block(j):
            so = 2 * j
            S.dma("sp", lambda e, j=j: e.dma_start(out=xn.ap, in_=x_oth[j * 128:(j + 1) * 128, :]), [], [xn], xo_ctr)
            rms_to_hT(xn, 0, (hT.ap, hT), cs=CS_OTH)
            kv_ps = PS(5)
            for c in range(8):
                S.op("pe", lambda e, c=c: e.matmul(kv_ps.ap[:, 0:200], hT.ap[:, c, :], win.ap[:, c, 1024:1224], start=(c == 0), stop=(c == 7), skip_group_check=True), [hT, win], [kv_ps])
            uo_v = kv_ps.ap[:, 256:320].rearrange("p (g t) -> p g t", g=4)
            for gg in range(4):
                for c in range(8):
                    S.op("pe", lambda e, c=c, gg=gg: e.matmul(uo_v[:, gg, :], win.ap[:, c, 1224 + gg * 128:1224 + (gg + 1) * 128], hT.ap[:, c, 112:128],
                                                              start=False, stop=(c == 7), skip_group_check=True), [hT, win], [kv_ps])
            if j == 0:
                S.op("dve", lambda e: e.tensor_scalar(out=tailt.ap, in0=uo_v, scalar1=metat.ap[:, 1:2], scalar2=None, op0=ALU.mult), [kv_ps, metat], [tailt])
            else:
                S.op("act", lambda e: e.copy(out=tailt.ap, in_=uo_v), [kv_ps], [tailt])
            kside(so, kv_ps, cs=CS_OTH)


        out_blocks = []
        pend_router_prev = []
        for j in range(NB):
            g = j % GB
            so = 2 * j
            sw = 2 * j + 1
            if j == 0:
                other_block(0)
            S.stage(2)

            xa = acc[g]
            if j % GB == 0:
                S.dma("sp", lambda e, j=j, xa=xa: e.dma_start(out=xa.ap, in_=x_own[j * 128:(j + 1) * 128, :]), [], [xa], x_ctr[g])
            rms_to_hT(xa, 0, (hT.ap, hT))
            if j == 0:
                dump("hT0", hT)
            q_ps = PS(0)
            qi_ps = PS(1)
            kv_ps = PS(5)
            u_ps = PS(4)
            for c in range(8):
                S.op("pe", lambda e, c=c: e.matmul(q_ps.ap, hT.ap[:, c, :], win.ap[:, c, 0:512], start=(c == 0), stop=(c == 7)), [hT, win], [q_ps])
            for c in range(8):
                S.op("pe", lambda e, c=c: e.matmul(qi_ps.ap, hT.ap[:, c, :], win.ap[:, c, 512:1024], start=(c == 0), stop=(c == 7)), [hT, win], [qi_ps])
            for c in range(8):
                S.op("pe", lambda e, c=c: e.matmul(kv_ps.ap[:, 0:200], hT.ap[:, c, :], win.ap[:, c, 1024:1224], start=(c == 0), stop=(c == 7)), [hT, win], [kv_ps])
            u_v = u_ps.ap.rearrange("p (g t) -> p g t", g=4)
            for gg in range(4):
                for c in range(8):
                    S.op("pe", lambda e, c=c, gg=gg: e.matmul(u_v[:, gg, :], win.ap[:, c, 1224 + gg * 128:1224 + (gg + 1) * 128], hT.ap[:, c, :],
                                                              start=(c == 0 and gg == 0), stop=(c == 7), skip_group_check=True), [hT, win], [u_ps])
            head_norm((q_ps.ap, q_ps), 8, gq8.ap, qn)
            rope(qn.ap, qn, 8, sw, QQ.ap[:, :, 0:64], QQ)
            rope(qi_ps.ap.rearrange("p (h d) -> p h d", h=8), qi_ps, 8, sw, QQ.ap[:, :, 64:128], QQ)
            S.op("dve", lambda e: e.tensor_scalar(out=wsc.ap, in0=kv_ps.ap[:, 192:200], scalar1=float(8 ** -0.5 * 64 ** -0.5), scalar2=None, op0=ALU.mult), [kv_ps], [wsc])
            kside(sw, kv_ps)
            qt = PS(2)
            qtv = qt.ap.bitcast(BF16).rearrange("p (h t) -> p h t", h=8)
            for h in range(8):
                S.op("pe", lambda e, h=h: e.transpose(qtv[:, h, :], QQ.ap[:, h, :], ident_b.ap), [QQ, ident_b], [qt])
            S.op("act", lambda e: e.copy(out=QQT.ap, in_=qtv), [qt], [QQT])
            if j == 0:
                dump("QQT0", QQT)
                dump("kk", T(kk.ap[:, 0:256], kk.bufs[0:2]))
            S.stage(3)
            S.op("act", lambda e: e.copy(out=ext.ap[:, :, 16:144], in_=u_v), [u_ps], [ext])
            S.defer_begin()
            S.op("pool", lambda e: e.tensor_copy(out=ext.ap[:, :, 0:16], in_=tailt.ap), [tailt], [ext])
            S.op("pool", lambda e: e.tensor_tensor(out=pA.ap[:, 0:4, 1:144], in0=ext.ap[:, 0:4, 1:144], in1=ext.ap[:, 0:4, 0:143], op=ALU.add), [ext], [pA])
            S.op("pool", lambda e: e.tensor_tensor(out=pB.ap[:, 1:4, 3:144], in0=pA.ap[:, 1:4, 3:144], in1=pA.ap[:, 1:4, 1:142], op=ALU.add), [pA], [pB])
            S.op("pool", lambda e: e.tensor_tensor(out=pA.ap[:, 2:4, 7:144], in0=pB.ap[:, 2:4, 7:144], in1=pB.ap[:, 2:4, 3:140], op=ALU.add), [pB], [pA])
            S.op("pool", lambda e: e.tensor_tensor(out=pB.ap[:, 3:4, 15:144], in0=pA.ap[:, 3:4, 15:144], in1=pA.ap[:, 3:4, 7:136], op=ALU.add), [pA], [pB])
            for gg, src in ((0, pA), (1, pB), (2, pA), (3, pB)):
                wv = src.ap[:, gg, 16:144]
                if j == 0:
                    S.op("pool", lambda e, wv=wv, gg=gg: e.tensor_tensor(out=wv, in0=wv, in1=rc0.ap[:, gg, :], op=ALU.mult), [src, rc0], [src])
                else:
                    S.op("pool", lambda e, wv=wv, gg=gg: e.tensor_scalar(out=wv, in0=wv, scalar1=1.0 / (2 << gg), scalar2=None, op0=ALU.mult), [src], [src])
                S.op("pool", lambda e, wv=wv, gg=gg: e.tensor_tensor(out=pooledT.ap[:, gg, :], in0=wv, in1=ext.ap[:, gg, 16:144], op=ALU.subtract), [src, ext], [pooledT])
            pm_ps = PS(3)
            for gg in range(4):
                S.op("pe", lambda e, gg=gg: e.matmul(pm_ps.ap[:, gg * 128:(gg + 1) * 128], pooledT.ap[:, gg, :], wpool.ap[:, gg, :], start=(gg == 0), stop=True, skip_group_check=True), [pooledT, wpool], [pm_ps])
            S.op("dve", lambda e: e.tensor_tensor(out=mixin.ap[:, 512:1024], in0=pm_ps.ap, in1=pscale_bc.ap, op=ALU.mult), [pm_ps, rowb], [mixin])
            pend_pool = S.defer_end()

            S.stage(4)
            if (j + 1) % GB != 0 and j + 1 < NB:
                xnx = acc[(j + 1) % GB]
                S.dma("sp", lambda e, j=j, xnx=xnx: e.dma_start(out=xnx.ap, in_=x_own[(j + 1) * 128:(j + 2) * 128, :]), [], [xnx], x_ctr[(j + 1) % GB])
            nslots = 2 * j + 2
            nk = nslots * 128
            for s in range(nslots):
                sp_ = PS((s % 3) * 2, 2)
                spv = sp_.ap.rearrange("p (h k) -> p h k", h=8)
                rl = relu[s % 3]
                for h in range(8):
                    S.op("pe", lambda e, h=h, s=s, spv=spv: e.matmul(spv[:, h, :], QQT.ap[64:128, h, :], kk.ap[64:128, s * 128:(s + 1) * 128],
                                                                     start=(h % 4 == 0), stop=True, skip_group_check=True), [QQT, kk.bufs[s]], [sp_])
                S.op("act", lambda e, rl=rl, spv=spv: e.activation(out=rl.ap, in_=spv.rearrange("p h k -> p k h"), func=AF.Relu), [sp_], [rl])
                S.op("pool" if s % 3 != 2 else "dve", lambda e, rl=rl: e.tensor_tensor(out=rl.ap, in0=rl.ap, in1=wsc.ap.unsqueeze(1).to_broadcast([128, 128, 8]), op=ALU.mult), [rl, wsc], [rl])
                Ic = It.ap[:, s * 128:(s + 1) * 128]
                Ib = It.bufs[s // 8]
                S.op("dve", lambda e, rl=rl, Ic=Ic: e.tensor_reduce(out=Ic, in_=rl.ap, axis=AX.X, op=ALU.add), [rl], [Ib])
            Ibs = It.bufs[0:(nslots + 7) // 8]
            S.op("dve", lambda e, nk=nk: e.tensor_reduce(out=c_B, in_=It.ap[:, 0:nk], axis=AX.X, op=ALU.max, apply_absolute_value=True), Ibs, [sm])
            S.op("dve", lambda e: e.tensor_scalar(out=It.ap[:, 0:128], in0=It.ap[:, 0:128], scalar1=metat.ap[:, 0:1], scalar2=None, op0=ALU.add), [It.bufs[0], metat], [It.bufs[0]])
            lastI = It.ap[:, (nslots - 1) * 128:nslots * 128]
            S.op("dve", lambda e, lastI=lastI: e.tensor_tensor(out=lastI, in0=lastI, in1=tri.ap, op=ALU.add), [It.bufs[(nslots - 1) // 8], tri], [It.bufs[(nslots - 1) // 8]])
            nd_slots = nslots if nslots < 4 else max(1, int(round(nslots * DVE_FRAC)))
            nd = nd_slots * 128
            na = nk - nd
            Ibd = It.bufs[0:(nd_slots + 7) // 8]
            Iba = It.bufs[nd_slots // 8:(nslots + 7) // 8]
            S.op("dve", lambda e: e.tensor_scalar(out=c_B, in0=c_B, scalar1=1.001, scalar2=1e-30, op0=ALU.mult, op1=ALU.add), [sm], [sm])
            S.op("dve", lambda e: e.tensor_scalar(out=steps.ap, in0=pow2_bc.ap, scalar1=c_B, scalar2=None, op0=ALU.mult), [sm, rowb], [steps])
            S.op("dve", lambda e: e.tensor_scalar(out=nsteps.ap, in0=steps.ap, scalar1=-1.0, scalar2=None, op0=ALU.mult), [steps], [nsteps])
            S.op("dve", lambda e: e.tensor_scalar(out=c_lo, in0=c_B, scalar1=-1.0, scalar2=None, op0=ALU.mult), [sm], [sm])
            thr = float(512 - na)
            pend = []
            if j + 1 < NB:
                S.defer_begin()
                other_block(j + 1)
                pend = S.defer_end()
            pend = pend_router_prev + pend_pool + pend
            per_it = (len(pend) + NIT - 1) // NIT
            for it in range(NIT):
                S.emit_some(pend, per_it)
                S.op("dve", lambda e, it=it: e.tensor_tensor(out=tst.ap[:, 0:1], in0=c_lo, in1=steps.ap[:, it:it + 1], op=ALU.add), [sm, steps], [tst])
                S.op("dve", lambda e, nd=nd: e.tensor_scalar(out=c_g.to_broadcast([128, nd]), in0=It.ap[:, 0:nd], scalar1=tst.ap[:, 0:1], scalar2=None,
                                                           op0=ALU.is_ge, op1=ALU.add, accum_out=c_cnt), [tst] + Ibd, [sm])
                if na > 0:
                    S.op("act", lambda e, nd=nd, nk=nk, na=na: e.activation(out=sm2.ap[:, 1:2].to_broadcast([128, na]), in_=It.ap[:, nd:nk], func=AF.Sign,
                                                                          bias=tst.ap[:, 0:1], scale=-1.0, accum_out=sm2.ap[:, 0:1]), [tst] + Iba, [sm2])
                    S.op("dve", lambda e: e.scalar_tensor_tensor(out=c_cnt, in0=c_cnt, scalar=2.0, in1=sm2.ap[:, 0:1], op0=ALU.mult, op1=ALU.subtract), [sm, sm2], [sm])
                    S.op("dve", lambda e, it=it, thr=thr: e.tensor_scalar(out=c_g, in0=c_cnt, scalar1=thr, scalar2=steps.ap[:, it:it + 1], op0=ALU.is_ge, op1=ALU.mult), [sm, steps], [sm])
                else:
                    S.op("dve", lambda e, it=it: e.tensor_scalar(out=c_g, in0=c_cnt, scalar1=256.0, scalar2=steps.ap[:, it:it + 1], op0=ALU.is_ge, op1=ALU.mult), [sm, steps], [sm])
                S.op("dve", lambda e: e.tensor_tensor(out=c_lo, in0=c_lo, in1=c_g, op=ALU.add), [sm], [sm])
            S.emit_some(pend, len(pend))
            if j in (0, 1, 5):
                dump(f"I{j}", T(It.ap[:, 0:nk], Ibs))
                dump(f"lo{j}", sm, ap=c_lo)

            S.stage(5)
            o_ps = PS(4, 2)
            oT = o_ps.ap[0:66, :]

            def emit_m01(s0):
                mbt = Mb[(s0 // 4) % 2]
                ns4 = min(4, nslots - s0)
                S.op("dve", lambda e, mbt=mbt, s0=s0, ns4=ns4: e.tensor_scalar(out=mbt.ap[:, 0:ns4 * 128], in0=It.ap[:, s0 * 128:(s0 + ns4) * 128], scalar1=c_lo, scalar2=None,
                                                                            op0=ALU.is_ge), [sm, It.bufs[s0 // 8]], [mbt])

            def emit_qk(s_):
                lt = PS((s_ % 2) * 2, 2)
                for half in range(2):
                    S.op("pe", lambda e, s_=s_, half=half, lt=lt: e.matmul(lt.ap[:, half * 512:(half + 1) * 512], kk.ap[:, s_ * 128:(s_ + 1) * 128],
                                                                          QQTz.ap[:, half * 512:(half + 1) * 512], start=True, stop=True),
                         [kk.bufs[s_], QQTz], [lt])
                mbt = Mb[(s_ // 4) % 2]
                mt = PS(6 + s_ % 2)
                S.op("pe", lambda e, mbt=mbt, mt=mt, s_=s_: e.transpose(mt.ap.bitcast(BF16)[:, 0:128], mbt.ap[:, (s_ % 4) * 128:(s_ % 4 + 1) * 128], ident_b.ap), [mbt, ident_b], [mt])

            S.op("pool", lambda e: e.memset(QQTz.ap[64:128, :], 0.0), [], [QQTz])
            S.op("pool", lambda e: e.tensor_copy(out=QQTz.ap[0:64, :], in_=QQT.ap[0:64, :, :].rearrange("p h t -> p (h t)")), [QQT], [QQTz])
            emit_m01(0)
            emit_qk(0)
            for s in range(nslots):
                if s + 1 < nslots:
                    if (s + 1) % 4 == 0:
                        emit_m01(s + 1)
                    emit_qk(s + 1)
                lt = PS((s % 2) * 2, 2)
                mt = PS(6 + s % 2)
                pt = PT[s % len(PT)]
                S.op("act", lambda e, pt=pt, lt=lt: e.activation(out=pt.ap, in_=lt.ap, func=AF.Exp), [lt], [pt])
                S.op("dve", lambda e, pt=pt, mt=mt: e.tensor_tensor(out=pt.ap.rearrange("p (h t) -> p h t", h=8), in0=pt.ap.rearrange("p (h t) -> p h t", h=8),
                                                                   in1=mt.ap.bitcast(BF16)[:, 0:128].unsqueeze(1).to_broadcast([128, 8, 128]), op=ALU.mult), [pt, mt], [pt])
                for half in range(2):
                    S.op("pe", lambda e, s=s, half=half, pt=pt: e.matmul(oT[:, half * 512:(half + 1) * 512], Vt.ap[:, s, :], pt.ap[:, half * 512:(half + 1) * 512],
                                                                        start=(s == 0), stop=(s == nslots - 1)), [pt, Vt.bufs[s]], [o_ps])
            oTs = xn.ap[0:66, :]
            S.op("act", lambda e: e.copy(out=oTs, in_=oT), [o_ps], [xn])
            o2 = PS(0, 2)
            o_v = o2.ap.rearrange("p (h d) -> p h d", h=8)
            for h in range(8):
                S.op("pe", lambda e, h=h: e.transpose(o_v[:, h, 0:66], oTs[:, h * 128:(h + 1) * 128], ident_f.ap[0:66, 0:66]), [xn, ident_f], [o2])
            o_ps = o2
            S.op("dve", lambda e: e.reciprocal(out=c_rden, in_=o_v[:, :, 64]), [o_ps], [sm])
            S.op("dve", lambda e: e.tensor_tensor(out=mixin.ap[:, 0:512].rearrange("p (h d) -> p h d", h=8), in0=o_v[:, :, 0:64],
                                                  in1=c_rden.unsqueeze(2).to_broadcast([128, 8, 64]), op=ALU.mult), [o_ps, sm], [mixin])
            if j in (0, 1, 5):
                dump(f"mixin{j}", mixin)

            S.stage(6)
            defer_router = (g != GB - 1)
            if defer_router:
                S.defer_begin()
            mt = PS(2)
            mtv = mt.ap.bitcast(BF16).rearrange("p (c t) -> p c t", c=8)
            for c in range(8):
                S.op("pe", lambda e, c=c: e.transpose(mtv[:, c, :], mixin.ap[:, c * 128:(c + 1) * 128], ident_b.ap), [mixin, ident_b], [mt])
            S.op("act", lambda e: e.copy(out=hT.ap, in_=mtv), [mt], [hT])
            y1 = PS(6, 2)
            for half in range(2):
                for c in range(8):
                    S.op("pe", lambda e, c=c, half=half: e.matmul(y1.ap[:, half * 512:(half + 1) * 512], hT.ap[:, c, :], wout.ap[:, c, half * 512:(half + 1) * 512],
                                                                  start=(c == 0), stop=(c == 7)), [hT, wout], [y1])
            S.op("dve", lambda e: e.tensor_tensor(out=xn.ap, in0=y1.ap, in1=G1.ap, op=ALU.mult), [y1, G1], [xn])
            S.op("pool", lambda e, xa=xa: e.tensor_tensor(out=xa.ap, in0=xa.ap, in1=xn.ap, op=ALU.add), [xa, xn], [xa])
            if j in (0, 1, 5):
                dump(f"x1_{j}", xa)

            S.stage(7)
            h2b = h2T.bufs[g]
            rms_to_hT(xa, 2, (h2T.ap[:, :, g * 128:(g + 1) * 128], h2b), dst_f32=h2Tf, cs=(CS_OTH if defer_router else None))
            lg_ps = PS(2)
            for c in range(8):
                S.op("pe", lambda e, c=c: e.matmul(lg_ps.ap[:, 0:36], h2Tf.ap[:, c, :], wr.ap[:, c, :], start=(c == 0), stop=(c == 7)), [h2Tf, wr], [lg_ps])
            S.op("dve", lambda e: e.tensor_tensor(out=lg.ap, in0=lg_ps.ap[:, 0:36], in1=brt_bc.ap, op=ALU.add), [lg_ps, rowb], [lg])
            R = rsm.ap
            r_gmax, r_ngmax, r_sumg, r_pg = R[:, 0:1], R[:, 1:2], R[:, 2:3], R[:, 3:4]
            r_ohg, r_eg, r_esel, r_m8 = R[:, 4:8], R[:, 8:12], R[:, 16:24], R[:, 24:32]
            r_d, r_ed, r_tp1, r_tp2 = R[:, 32:33], R[:, 33:34], R[:, 34:35], R[:, 35:36]
            r_w1, r_w2, r_t48 = R[:, 40:48], R[:, 48:56], R[:, 56:88]
            dd = ([lg, rsm], [rsm])
            S.op("dve", lambda e: e.tensor_reduce(out=r_gmax, in_=lg.ap[:, 0:4], axis=AX.X, op=ALU.max), *dd)
            S.op("dve", lambda e: e.tensor_scalar(out=r_ohg, in0=lg.ap[:, 0:4], scalar1=r_gmax, scalar2=None, op0=ALU.is_equal), *dd)
            S.op("dve", lambda e: e.tensor_scalar(out=r_ngmax, in0=r_gmax, scalar1=-1.0, scalar2=None, op0=ALU.mult), *dd)
            S.op("act", lambda e: e.activation(out=r_eg, in_=lg.ap[:, 0:4], func=AF.Exp, bias=r_ngmax, scale=1.0, accum_out=r_sumg), *dd)
            S.op("dve", lambda e: e.reciprocal(out=r_pg, in_=r_sumg), *dd)
            S.op("dve", lambda e: e.tensor_tensor(out=r_t48.rearrange("p (g x) -> p g x", g=4), in0=lg.ap[:, 4:36].rearrange("p (g x) -> p g x", g=4),
                                                  in1=r_ohg.unsqueeze(2).to_broadcast([128, 4, 8]), op=ALU.mult), *dd)
            S.op("dve", lambda e: e.tensor_reduce(out=r_esel, in_=r_t48.rearrange("p (g x) -> p x g", g=4), axis=AX.X, op=ALU.add), *dd)
            S.op("dve", lambda e: e.max(out=r_m8, in_=r_esel), *dd)
            S.op("dve", lambda e: e.tensor_tensor(out=r_d, in0=r_m8[:, 1:2], in1=r_m8[:, 0:1], op=ALU.subtract), *dd)
            S.op("act", lambda e: e.activation(out=r_ed, in_=r_d, func=AF.Exp), *dd)
            S.op("dve", lambda e: e.tensor_scalar(out=r_tp1, in0=r_ed, scalar1=1.0, scalar2=None, op0=ALU.add), *dd)
            S.op("dve", lambda e: e.reciprocal(out=r_tp1, in_=r_tp1), *dd)
            S.op("dve", lambda e: e.tensor_tensor(out=r_tp2, in0=r_ed, in1=r_tp1, op=ALU.mult), *dd)
            S.op("dve", lambda e: e.tensor_scalar(out=r_w1, in0=r_esel, scalar1=r_m8[:, 0:1], scalar2=r_tp1, op0=ALU.is_equal, op1=ALU.mult), *dd)
            S.op("dve", lambda e: e.tensor_scalar(out=r_w2, in0=r_esel, scalar1=r_m8[:, 1:2], scalar2=r_tp2, op0=ALU.is_equal, op1=ALU.mult), *dd)
            S.op("dve", lambda e: e.tensor_tensor(out=r_w1, in0=r_w1, in1=r_w2, op=ALU.add), *dd)
            S.op("dve", lambda e: e.tensor_scalar(out=r_ohg, in0=r_ohg, scalar1=r_pg, scalar2=None, op0=ALU.mult), *dd)
            gt_ = gates[g]
            S.op("dve", lambda e, gt_=gt_: e.tensor_tensor(out=gt_.ap.rearrange("p (g x) -> p g x", g=4), in0=r_ohg.unsqueeze(2).to_broadcast([128, 4, 8]),
                                                          in1=r_w1.unsqueeze(1).to_broadcast([128, 4, 8]), op=ALU.mult), [rsm], [gt_])
            if j in (0, 1, 5):
                dump(f"gates{j}", gt_)
            pend_router = S.defer_end() if defer_router else []
            pend_router_prev = pend_router
            out_blocks.append(j)

            S.stage(8)
            if g == GB - 1:
                S.barrier()
                A.reset(att_end if False else amark)
                accy = [A.tile(f"accy{i}", [D], F32) for i in range(GB)]
                sgh = [A.tile(f"sg{i}", [512], F32) for i in range(2)]
                aTh = [A.tile(f"aT{i}", [512], BF16) for i in range(2)]
                stg_g = A.tile("stg_g", [8, DE], F32)
                stg_u = A.tile("stg_u", [8, DE], F32)
                stg_d = A.tile("stg_d", [2, D], F32)
                wgu = [A.tile(f"wgu{i}", [8, 512], BF16) for i in range(2)]
                wd = [A.tile(f"wd{i}", [2, D], BF16) for i in range(2)]
                outst = A.tile("outst", [D], F32)
                tmpo = A.tile("tmpo", [D], F32)
                if j == GB - 1:
                    moe_ctrs = (S.dctr("wg"), S.dctr("wu"), S.dctr("wd"), [S.dctr("wgu0"), S.dctr("wgu1")], [S.dctr("wdb0"), S.dctr("wdb1")], S.dctr("wscr"))
                wg_ctr, wu_ctr, wd_ctr, wgu_ctr, wdb_ctr, wscr_ctr = moe_ctrs
                for ex in range(NE):
                    wgu_ = wgu[ex % 2]
                    wd_ = wd[ex % 2]
                    if j == GB - 1:
                        S.dma("sp", lambda e, ex=ex: e.dma_start(out=stg_g.ap, in_=w_gate[ex].rearrange("(kc p) f -> p kc f", p=128)), [], [stg_g], wg_ctr)
                        S.dma("sp", lambda e, ex=ex: e.dma_start(out=stg_u.ap, in_=w_up[ex].rearrange("(kc p) f -> p kc f", p=128)), [], [stg_u], wu_ctr)
                        S.dma("sp", lambda e, ex=ex: e.dma_start(out=stg_d.ap, in_=w_down[ex].rearrange("(kc p) n -> p kc n", p=128)), [], [stg_d], wd_ctr)
                        S.op("act", lambda e, wgu_=wgu_: e.copy(out=wgu_.ap[:, :, 0:256], in_=stg_g.ap), [stg_g], [wgu_])
                        S.op("pool", lambda e, wgu_=wgu_: e.tensor_copy(out=wgu_.ap[:, :, 256:512], in_=stg_u.ap), [stg_u], [wgu_])
                        S.op("dve", lambda e, wd_=wd_: e.tensor_copy(out=wd_.ap, in_=stg_d.ap), [stg_d], [wd_])
                        S.dma("sp", lambda e, ex=ex, wgu_=wgu_: e.dma_start(out=wgu_s[ex], in_=wgu_.ap.rearrange("p a b -> p (a b)")), [wgu_], [], wscr_ctr)
                        S.dma("sp", lambda e, ex=ex, wd_=wd_: e.dma_start(out=wd_s[ex], in_=wd_.ap.rearrange("p a b -> p (a b)")), [wd_], [], wscr_ctr)
                    else:
                        S.dma("sp", lambda e, ex=ex, wgu_=wgu_: e.dma_start(out=wgu_.ap.rearrange("p a b -> p (a b)"), in_=wgu_s[ex]), [], [wgu_], wgu_ctr[ex % 2])
                        S.dma("sp", lambda e, ex=ex, wd_=wd_: e.dma_start(out=wd_.ap.rearrange("p a b -> p (a b)"), in_=wd_s[ex]), [], [wd_], wdb_ctr[ex % 2])
                    gu = [PS(b) for b in range(4)]
                    for hh in range(2):
                        for oc in (hh, 2 + hh):
                            for c in range(8):
                                S.op("pe", lambda e, oc=oc, c=c, wgu_=wgu_: e.matmul(gu[oc].ap, wgu_.ap[:, c, oc * 128:(oc + 1) * 128], h2T.ap[:, c, :], start=(c == 0), stop=(c == 7)),
                                     [wgu_, h2T], [gu[oc]])
                        S.op("act", lambda e, hh=hh: e.activation(out=sgh[hh].ap, in_=gu[hh].ap, func=AF.Silu), [gu[hh]], [sgh[hh]])
                        S.op("dve", lambda e, hh=hh: e.tensor_tensor(out=aTh[hh].ap, in0=sgh[hh].ap, in1=gu[2 + hh].ap, op=ALU.mult), [sgh[hh], gu[2 + hh]], [aTh[hh]])
                    for pair in range(GB // 2):
                        blks = (2 * pair, 2 * pair + 1)
                        yps = {blk: PS(4 + (blk % 2) * 2, 2) for blk in blks}
                        for c2 in range(2):
                            for blk in blks:
                                for half in range(2):
                                    S.op("pe", lambda e, half=half, c2=c2, blk=blk, yp=yps[blk], wd_=wd_: e.matmul(yp.ap[:, half * 512:(half + 1) * 512], aTh[c2].ap[:, blk * 128:(blk + 1) * 128],
                                                                                                                wd_.ap[:, c2, half * 512:(half + 1) * 512], start=(c2 == 0), stop=(c2 == 1)),
                                         [aTh[c2], wd_], [yps[blk]])
                        for blk in blks:
                            yp = yps[blk]
                            gcol = gates[blk].ap[:, ex:ex + 1]
                            if ex == 0:
                                S.op("dve", lambda e, blk=blk, yp=yp, gcol=gcol: e.tensor_scalar(out=accy[blk].ap, in0=yp.ap, scalar1=gcol, scalar2=None, op0=ALU.mult), [yp, gates[blk]], [accy[blk]])
                            else:
                                S.op("dve", lambda e, blk=blk, yp=yp, gcol=gcol: e.scalar_tensor_tensor(out=accy[blk].ap, in0=yp.ap, scalar=gcol, in1=accy[blk].ap, op0=ALU.mult, op1=ALU.add),
                                     [yp, gates[blk], accy[blk]], [accy[blk]])
                for blk in range(GB):
                    jb = j - (GB - 1) + blk
                    S.op("dve", lambda e, blk=blk: e.tensor_tensor(out=tmpo.ap, in0=accy[blk].ap, in1=G2.ap, op=ALU.mult), [accy[blk], G2], [tmpo])
                    S.op("dve" if blk % 2 == 0 else "pool", lambda e, blk=blk: e.tensor_tensor(out=outst.ap, in0=tmpo.ap, in1=acc[blk].ap, op=ALU.add), [tmpo, acc[blk]], [outst])
                    S.dma("sp", lambda e, jb=jb: e.dma_start(out=out_d[jb * 128:(jb + 1) * 128, :], in_=outst.ap), [outst], [], out_ctr[blk])
                S.barrier()
                A.reset(att_end)

        S.final_wait("sp")

        block = es.enter_context(nc.Block())

        @block.sync
        def _(e):
            S.replay("sp", e)

        @block.tensor
        def _(e):
            S.replay("pe", e)

        @block.scalar
        def _(e):
            S.replay("act", e)

        @block.vector
        def _(e):
            S.replay("dve", e)

        @block.gpsimd
        def _(e):
            S.replay("pool", e)

    return nc


def _prep_inputs(x, c, positions, w_ada, b_ada, g_norm_mix, g_norm_ffn, w_in, g_q, g_k, g_kidx,
                 w_pool, pool_scale, w_out, w_router_group, b_router_group, w_router_expert,
                 b_router_expert, w_gate, w_up, w_down, cores=range(8)):
    f = np.float32
    x = np.asarray(x, f)
    W = np.asarray(w_in[0], f)
    perm = np.concatenate([np.arange(0, 512), np.arange(640, 1152), np.arange(512, 576), np.arange(1152, 1216),
                           np.arange(576, 640), np.arange(1216, 1224), np.arange(1224, 1736)])
    w_in_p = np.ascontiguousarray(W[:, perm])
    inv_freq = (np.float32(10000.0) ** (-np.arange(0, 64, 2, dtype=np.float32) / np.float32(64))).astype(f)
    pow2 = (2.0 ** -np.arange(24)).astype(f)
    rowp = np.zeros((1, 832), f)
    rowp[0, 0:64] = g_q[0]
    rowp[0, 64:128] = g_k[0]
    rowp[0, 128:192] = g_kidx[0]
    rowp[0, 192:704] = pool_scale[0]
    rowp[0, 704:708] = b_router_group[0]
    rowp[0, 708:740] = b_router_expert[0]
    rowp[0, 740:772] = inv_freq
    rowp[0, 772:796] = pow2
    bg = np.concatenate([b_ada[0][2048:3072], b_ada[0][5120:6144]]).astype(f)[None, :]
    w_r = np.ascontiguousarray(np.concatenate([w_router_group[0], w_router_expert[0]], axis=1).astype(f))
    wp = np.ascontiguousarray(np.asarray(w_pool[0], f).transpose(1, 0, 2).reshape(128, 512))
    shared = {
        "rowp": rowp, "bgate": np.ascontiguousarray(bg), "w_ada": np.ascontiguousarray(w_ada[0], f),
        "w_in": w_in_p, "w_out": np.ascontiguousarray(w_out[0], f), "w_pool": wp, "w_r": w_r,
        "w_gate": np.ascontiguousarray(w_gate[0], f), "w_up": np.ascontiguousarray(w_up[0], f),
        "w_down": np.ascontiguousarray(w_down[0], f),
    }
    in_maps = []
    wins = np.array([2, 4, 8, 16], f)
    for core in cores:
        b, p = core // 2, core % 2
        xb = x[b].reshape(64, 128, D)
        pb = np.asarray(positions[b]).reshape(64, 128).astype(np.int32)
        own = xb[p::2]
        pos_own = pb[p::2]
        if p == 1:
            oth = xb[0::2]
            pos_oth = pb[0::2]
        else:
            oth = np.concatenate([np.zeros((1, 128, D), f), xb[1:63:2]], axis=0)
            pos_oth = np.concatenate([np.zeros((1, 128), np.int32), pb[1:63:2]], axis=0)
        pos = np.zeros((128, 64), np.int32)
        pos[:, 0::2] = pos_oth.T
        pos[:, 1::2] = pos_own.T
        colp = np.zeros((128, 72), f)
        colp[:, 0:8] = np.asarray(c[b], f).reshape(8, 128).T
        colp[:, 8:16] = np.asarray(g_norm_mix[0], f).reshape(8, 128).T
        colp[:, 16:24] = np.asarray(g_norm_ffn[0], f).reshape(8, 128).T
        colp[:, 24:72] = np.asarray(b_ada[0], f).reshape(48, 128).T
        meta = np.zeros((128, 2), f)
        meta[:, 0] = 0.0 if p == 1 else NEG
        meta[:, 1] = float(p)
        t = np.arange(128, dtype=f)
        if p == 0:
            cnt = np.minimum(t[None, :] + 1.0, wins[:, None])
        else:
            cnt = np.broadcast_to(wins[:, None], (4, 128))
        rc = np.broadcast_to((1.0 / cnt).astype(f).reshape(1, 512), (128, 512))
        m = dict(shared)
        m.update({
            "x_own": np.ascontiguousarray(own.reshape(-1, D)),
            "x_oth": np.ascontiguousarray(oth.reshape(-1, D)),
            "pos": pos, "colp": colp, "meta": meta, "rc0": np.ascontiguousarray(rc),
        })
        in_maps.append(m)
    return in_maps


_NC_CACHE = {}


def kernel(**inputs):
    if "nc" not in _NC_CACHE:
        _NC_CACHE["nc"] = build_program()
    nc = _NC_CACHE["nc"]
    in_maps = _prep_inputs(**inputs)
    res = run_bass_kernel_spmd(nc, in_maps, core_ids=list(range(8)))
    out = np.zeros((4, 64, 128, D), np.float32)
    for core in range(8):
        b, p = core // 2, core % 2
        out[b, p::2] = np.asarray(res.results[core]["out"]).reshape(NB, 128, D)
    return out.reshape(4, 8192, D)
```

```python
import numpy as np
from contextlib import ExitStack
import concourse.bass as bass
import concourse.mybir as mybir
from concourse.bass_utils import run_bass_kernel_spmd

F32 = mybir.dt.float32
BF16 = mybir.dt.bfloat16
I32 = mybir.dt.int32
U8 = mybir.dt.uint8
AF = mybir.ActivationFunctionType
ALU = mybir.AluOpType
AX = mybir.AxisListType

D = 1024
NB = 32
NSLOT = 64
GB = 4
NIT = 22
DVE_FRAC = 0.45
NE = 32
DE = 256
EPS = 1e-6
NEG = -3.0e38
MNEG = -30000.0
ARENA_KB = 206

ENGS = ("pe", "act", "dve", "pool", "sp")
NO_SELF = ("pe", "sp")

DEBUG = {}
STOP_AT = [99]


class Buf:
    __slots__ = ("name", "w", "rs", "excl")

    def __init__(self, name, excl=False):
        self.name = name
        self.w = None
        self.rs = {}
        self.excl = excl


class Ctr:
    __slots__ = ("sem", "n", "name")

    def __init__(self, sem, name):
        self.sem = sem
        self.n = 0
        self.name = name


class T:
    def __init__(self, ap, bufs):
        self.ap = ap
        self.bufs = bufs if isinstance(bufs, (list, tuple)) else [bufs]


def _bufs(xs):
    out = []
    for x in xs:
        if x is None:
            continue
        if isinstance(x, Buf):
            out.append(x)
        elif isinstance(x, T):
            out.extend(x.bufs)
        else:
            out.extend(_bufs(x))
    return out


class Sched:
    def __init__(self, nc, es):
        self.nc = nc
        self.es = es
        self.q = {e: [] for e in ENGS}
        self.nsem = 0
        self.ectr = {e: self.ctr("eng_" + e) for e in ENGS}
        self.waited = {e: {} for e in ENGS}
        self.dma_ctrs = []
        self.stopped = False
        self.deferred = None

    def stage(self, n):
        if STOP_AT[0] <= n:
            self.stopped = True

    def ctr(self, name):
        self.nsem += 1
        return Ctr(self.es.enter_context(self.nc.semaphore("s_" + name)), name)

    def dctr(self, name):
        c = self.ctr(name)
        self.dma_ctrs.append(c)
        return c

    def _collect(self, eng, rb, wb, extra=()):
        need = {}
        me = self.ectr[eng]
        wd = self.waited[eng]

        def add(c, v):
            if c is me and eng in NO_SELF:
                return
            if wd.get(c, 0) >= v:
                return
            if need.get(c, 0) < v:
                need[c] = v

        for b in rb:
            if b.w is not None:
                add(*b.w)
        for b in wb:
            if b.w is not None:
                add(*b.w)
            for c, v in b.rs.items():
                add(c, v)
        for c, v in extra:
            add(c, v)
        for c, v in need.items():
            wd[c] = v
        return [(c.sem, v) for c, v in need.items()]

    def defer_begin(self):
        self.deferred = []

    def defer_end(self):
        d = self.deferred
        self.deferred = None
        return d

    def emit_some(self, pend, n):
        while pend and n > 0:
            kind, args = pend.pop(0)
            (self.op if kind == "op" else self.dma)(*args)
            n -= 1

    def op(self, eng, fn, reads=(), writes=()):
        if self.stopped:
            return
        if self.deferred is not None:
            self.deferred.append(("op", (eng, fn, reads, writes)))
            return
        rb = _bufs(reads)
        wb = _bufs(writes)
        if any(b.excl for b in rb):
            wb = wb + [b for b in rb if b.excl]
            rb = [b for b in rb if not b.excl]
        waits = self._collect(eng, rb, wb)
        c = self.ectr[eng]
        c.n += 1
        self.q[eng].append((waits, fn, c.sem, 1))
        for b in rb:
            if b.rs.get(c, 0) < c.n:
                b.rs[c] = c.n
        for b in wb:
            b.w = (c, c.n)
            b.rs = {}

    def dma(self, eng, fn, reads, writes, ctr):
        if self.stopped:
            return
        if self.deferred is not None:
            self.deferred.append(("dma", (eng, fn, reads, writes, ctr)))
            return
        rb = _bufs(reads)
        wb = _bufs(writes)
        waits = self._collect(eng, rb, wb)
        ctr.n += 16
        self.q[eng].append((waits, fn, ctr.sem, 16))
        for b in rb:
            if b.rs.get(ctr, 0) < ctr.n:
                b.rs[ctr] = ctr.n
        for b in wb:
            b.w = (ctr, ctr.n)
            b.rs = {}

    def barrier(self):
        if self.stopped:
            return
        snap = [(self.ectr[e], self.ectr[e].n) for e in ENGS if self.ectr[e].n > 0]
        snap += [(c, c.n) for c in self.dma_ctrs if c.n > 0]
        for e in ENGS:
            waits = self._collect(e, [], [], extra=snap)
            if waits:
                self.q[e].append((waits, None, None, 0))

    def final_wait(self, eng="sp"):
        self.stopped = False
        snap = [(self.ectr[e], self.ectr[e].n) for e in ENGS if self.ectr[e].n > 0 and e != eng]
        snap += [(c, c.n) for c in self.dma_ctrs if c.n > 0]
        waits = self._collect(eng, [], [], extra=snap)
        if waits:
            self.q[eng].append((waits, None, None, 0))

    def replay(self, eng, e):
        for waits, fn, sem, inc in self.q[eng]:
            for s, v in waits:
                e.wait_ge(s, v)
            if fn is not None:
                ins = fn(e)
                ins.then_inc(sem, inc)


class Arena:
    def __init__(self, ap_u8, nbytes):
        self.ap = ap_u8
        self.nbytes = nbytes
        self.off = 0

    def mark(self):
        return self.off

    def reset(self, m):
        self.off = m

    def tile(self, name, free_shape, dt, nbufs=None, parts=128):
        esz = {F32: 4, BF16: 2, I32: 4, U8: 1}[dt]
        n = 1
        for s in free_shape:
            n *= s
        nb = n * esz
        self.off = (self.off + 63) // 64 * 64
        assert self.off + nb <= self.nbytes, f"arena overflow at {name}: {self.off + nb} > {self.nbytes}"
        ap = self.ap[:, self.off:self.off + nb]
        self.off += nb
        if dt != U8:
            ap = ap.bitcast(dt)
        if len(free_shape) == 2:
            ap = ap.rearrange("p (a b) -> p a b", a=free_shape[0])
        elif len(free_shape) == 3:
            ap = ap.rearrange("p (a b c) -> p a b c", a=free_shape[0], b=free_shape[1])
        if nbufs is None:
            bufs = [Buf(name)]
        else:
            bufs = [Buf(f"{name}{i}") for i in range(nbufs)]
        return T(ap, bufs)


def build_program(dbg=None):
    dbg = dbg or {}
    nc = bass.Bass("TRN2", target_bir_lowering=False, dynamic_dma_scratch_size=1024)
    dram = {}

    def din(name, shape, dt=F32):
        dram[name] = nc.dram_tensor(name, list(shape), dt, kind="ExternalInput").ap()
        return dram[name]

    x_own = din("x_own", [NB * 128, D])
    x_oth = din("x_oth", [NB * 128, D])
    pos_in = din("pos", [128, NSLOT], I32)
    colp = din("colp", [128, 72])
    rowp = din("rowp", [1, 832])
    bgate = din("bgate", [1, 2048])
    meta = din("meta", [128, 2])
    rc0_in = din("rc0", [128, 4 * 128])
    w_ada = din("w_ada", [D, 6 * D])
    w_in = din("w_in", [D, 1736])
    w_out = din("w_out", [D, D])
    w_pool = din("w_pool", [128, 4 * 128])
    w_r = din("w_r", [D, 36])
    w_gate = din("w_gate", [NE, D, DE])
    w_up = din("w_up", [NE, D, DE])
    w_down = din("w_down", [NE, DE, D])
    out_d = nc.dram_tensor("out", [NB * 128, D], F32, kind="ExternalOutput").ap()
    wgu_s = nc.dram_tensor("wgu_s", [NE, 128, 8 * 512], BF16, kind="Internal").ap()
    wd_s = nc.dram_tensor("wd_s", [NE, 128, 2 * D], BF16, kind="Internal").ap()
    dbg_d = {}
    for k, (shape, dt) in dbg.items():
        dbg_d[k] = nc.dram_tensor("dbg_" + k, list(shape), dt, kind="ExternalOutput").ap()

    with ExitStack() as es:
        arena_t = es.enter_context(nc.sbuf_tensor("arena", [128, ARENA_KB * 1024], U8))
        ps_t = es.enter_context(nc.psum_tensor("ps", [128, 8, 512], F32))
        S = Sched(nc, es)
        A = Arena(arena_t[:, :], ARENA_KB * 1024)
        pbank = [Buf(f"bank{i}", excl=True) for i in range(8)]

        def PS(b0, nb=1):
            ap = ps_t[:, b0:b0 + nb, :].rearrange("p a b -> p (a b)") if nb > 1 else ps_t[:, b0, :]
            return T(ap, pbank[b0:b0 + nb])

        dbg_ctr = S.dctr("dbg") if dbg else None

        def dump(name, t, ap=None):
            if name not in dbg_d:
                return
            src = ap if ap is not None else t.ap
            S.dma("sp", lambda e, o=dbg_d[name], i=src: e.dma_start(out=o, in_=i), [t], [], dbg_ctr)

        ident_f = A.tile("ident_f", [128], F32)
        ident_b = A.tile("ident_b", [128], BF16)
        irep = A.tile("irep", [4, 128], BF16)
        tri = A.tile("tri", [128], F32)
        ab = A.tile("ab", [4, 8], F32)
        G1 = A.tile("G1", [D], F32)
        G2 = A.tile("G2", [D], F32)
        rowb = A.tile("rowb", [832], F32)
        gq8 = A.tile("gq8", [64], F32)
        metat = A.tile("metat", [2], F32)
        rc0 = A.tile("rc0", [4, 128], F32)
        win = A.tile("win", [8, 1736], BF16)
        wout = A.tile("wout", [8, D], BF16)
        wpool = A.tile("wpool", [4, 128], BF16)
        wr = A.tile("wr", [8, 36], F32)
        kk = A.tile("kk", [NSLOT * 128], BF16, nbufs=NSLOT)
        Vt = A.tile("V", [NSLOT, 66], BF16, nbufs=NSLOT)
        sint = A.tile("sin", [NSLOT, 32], F32)
        cost = A.tile("cos", [NSLOT, 32], F32)
        acc = [A.tile(f"acc{g}", [D], F32) for g in range(GB)]
        h2T = A.tile("h2T", [8, GB * 128], BF16, nbufs=GB)
        gates = [A.tile(f"gates{g}", [NE], F32) for g in range(GB)]
        tailt = A.tile("tailt", [4, 16], F32)
        pmark = A.mark()
        cols = A.tile("cols", [72], F32)
        modc = A.tile("modc", [48], F32)
        sc2 = A.tile("sc2", [8, 2], F32)

        gk_bc = T(rowb.ap[:, 64:192], rowb.bufs)
        pscale_bc = T(rowb.ap[:, 192:704], rowb.bufs)
        brt_bc = T(rowb.ap[:, 704:740], rowb.bufs)
        invf_bc = T(rowb.ap[:, 740:772], rowb.bufs)
        pow2_bc = T(rowb.ap[:, 772:796], rowb.bufs)

        setup_ctr = S.dctr("setup")

        def ld(t, src, ap=None, ctr=None):
            dst = ap if ap is not None else t.ap
            S.dma("sp", lambda e, o=dst, i=src: e.dma_start(out=o, in_=i), [], [t], ctr or setup_ctr)

        ld(cols, colp)
        ld(rowb, rowp.partition_broadcast(128).rearrange("p a b -> p (a b)"))
        ld(metat, meta)
        ld(rc0, rc0_in.rearrange("p (a b) -> p a b", a=4))
        ld(wr, w_r.rearrange("(kc p) n -> p kc n", p=128))
        st_pos = A.tile("st_pos", [NSLOT], I32)
        ld(st_pos, pos_in)
        st_bg = A.tile("st_bg", [2048], F32, parts=1)
        ld(st_bg, bgate, ap=st_bg.ap[0:1, :])
        fin = (setup_ctr, setup_ctr.n)
        for t in (cols, rowb, metat, rc0, wr, st_pos, st_bg):
            t.bufs[0].w = fin

        io_i = A.tile("io_i", [128], I32)
        io_f = A.tile("io_f", [128], F32)
        S.op("pool", lambda e: e.iota(io_i.ap, pattern=[[1, 128]], base=0, channel_multiplier=-1), [], [io_i])
        S.op("dve", lambda e: e.tensor_copy(out=io_f.ap, in_=io_i.ap), [io_i], [io_f])
        S.op("dve", lambda e: e.tensor_scalar(out=ident_f.ap, in0=io_f.ap, scalar1=0.0, scalar2=None, op0=ALU.is_equal), [io_f], [ident_f])
        S.op("dve", lambda e: e.tensor_copy(out=ident_b.ap, in_=ident_f.ap), [ident_f], [ident_b])
        S.op("dve", lambda e: e.tensor_copy(out=irep.ap, in_=ident_f.ap.unsqueeze(1).to_broadcast([128, 4, 128])), [ident_f], [irep])
        S.op("dve", lambda e: e.tensor_scalar(out=tri.ap, in0=io_f.ap, scalar1=0.0, scalar2=NEG, op0=ALU.is_gt, op1=ALU.mult), [io_f], [tri])
        ones1 = A.tile("ones1", [128], F32, parts=1)
        S.op("dve", lambda e: e.memset(ones1.ap[0:1, :], 1.0), [], [ones1])
        S.op("dve", lambda e: e.memset(Vt.ap[:, :, 64:65], 1.0), [], [Vt])
        S.op("dve", lambda e: e.memset(Vt.ap[:, :, 65:66], 0.0), [], [Vt])
        S.op("dve", lambda e: e.tensor_scalar(out=gq8.ap, in0=rowb.ap[:, 0:64], scalar1=0.125, scalar2=None, op0=ALU.mult), [rowb], [gq8])

        S.op("act", lambda e: e.activation(out=sc2.ap[:, :, 0], in_=cols.ap[:, 0:8], func=AF.Silu), [cols], [sc2])
        S.op("act", lambda e: e.activation(out=sc2.ap[:, :, 1], in_=cols.ap[:, 0:8], func=AF.Silu), [cols], [sc2])
        scb = A.tile("scb", [8, 128], F32)
        S.op("dve", lambda e: e.tensor_copy(out=scb.ap, in_=sc2.ap[:, :, 0:1].to_broadcast([128, 8, 128])), [sc2], [scb])

        stg = [A.tile(f"stg{i}", [8, 512], F32) for i in range(2)]
        stg_ctr = [S.dctr(f"stg{i}") for i in range(2)]
        wada_v = w_ada.rearrange("(kc p) n -> p kc n", p=128)
        mod_ps = PS(0)
        mod_v = mod_ps.ap[:, 0:96].rearrange("p (a b) -> p a b", b=2)
        first_mod = True
        for i in range(12):
            st = stg[i % 2]
            S.dma("sp", lambda e, o=st.ap, s=wada_v[:, :, i * 512:(i + 1) * 512]: e.dma_start(out=o, in_=s), [], [st], stg_ctr[i % 2])
            for sub in range(4):
                col = i * 4 + sub
                for kc in range(8):
                    S.op("pe", lambda e, o=mod_v[:, col, :], l=st.ap[:, kc, sub * 128:(sub + 1) * 128], r=sc2.ap[:, kc, :], f=first_mod, kc=kc:
                         e.matmul(o, l, r, start=f, stop=(kc == 7), skip_group_check=True), [st, sc2], [mod_ps])
                    first_mod = False
            if i in (4, 5, 10, 11):
                gt = G1 if i < 6 else G2
                half = i % 2
                boff = (0 if i < 6 else 1024) + half * 512
                gps = PS(1 + (i % 2))
                for kc in range(8):
                    S.op("pe", lambda e, o=gps.ap, l=scb.ap[:, kc, :], r=st.ap[:, kc, :], kc=kc:
                         e.matmul(o, l, r, start=(kc == 0), stop=False), [st, scb], [gps])
                S.op("pe", lambda e, o=gps.ap, l=ones1.ap[0:1, :], r=st_bg.ap[0:1, boff:boff + 512]:
                     e.matmul(o, l, r, start=False, stop=True), [ones1, st_bg], [gps])
                S.op("act", lambda e, o=gt.ap[:, half * 512:(half + 1) * 512], i_=gps.ap: e.copy(out=o, in_=i_), [gps], [gt])
        S.op("dve", lambda e: e.tensor_tensor(out=modc.ap, in0=mod_v[:, :, 0], in1=cols.ap[:, 24:72], op=ALU.add), [mod_ps, cols], [modc])
        S.op("dve", lambda e: e.scalar_tensor_tensor(out=ab.ap[:, 0, :], in0=modc.ap[:, 8:16], scalar=1.0, in1=cols.ap[:, 8:16], op0=ALU.add, op1=ALU.mult), [modc, cols], [ab])
        S.op("dve", lambda e: e.tensor_copy(out=ab.ap[:, 1, :], in_=modc.ap[:, 0:8]), [modc], [ab])
        S.op("dve", lambda e: e.scalar_tensor_tensor(out=ab.ap[:, 2, :], in0=modc.ap[:, 32:40], scalar=1.0, in1=cols.ap[:, 16:24], op0=ALU.add, op1=ALU.mult), [modc, cols], [ab])
        S.op("dve", lambda e: e.tensor_copy(out=ab.ap[:, 3, :], in_=modc.ap[:, 24:32]), [modc], [ab])
        dump("ab", ab)
        dump("G1", G1)

        win_v = w_in.rearrange("(kc p) n -> p kc n", p=128)
        k = 0
        for (c0, c1) in ((0, 512), (512, 1024), (1024, 1536), (1536, 1736)):
            st = stg[k % 2]
            S.dma("sp", lambda e, o=st.ap[:, :, 0:c1 - c0], s=win_v[:, :, c0:c1]: e.dma_start(out=o, in_=s), [], [st], stg_ctr[k % 2])
            S.op("pool", lambda e, o=win.ap[:, :, c0:c1], i_=st.ap[:, :, 0:c1 - c0]: e.tensor_copy(out=o, in_=i_), [st], [win])
            k += 1
        wout_v = w_out.rearrange("(kc p) n -> p kc n", p=128)
        for (c0, c1) in ((0, 512), (512, 1024)):
            st = stg[k % 2]
            S.dma("sp", lambda e, o=st.ap, s=wout_v[:, :, c0:c1]: e.dma_start(out=o, in_=s), [], [st], stg_ctr[k % 2])
            S.op("pool", lambda e, o=wout.ap[:, :, c0:c1], i_=st.ap: e.tensor_copy(out=o, in_=i_), [st], [wout])
            k += 1
        st = stg[k % 2]
        S.dma("sp", lambda e, o=st.ap[:, 0, :], s=w_pool: e.dma_start(out=o, in_=s), [], [st], stg_ctr[k % 2])
        S.op("pool", lambda e, o=wpool.ap, i_=st.ap[:, 0, :].rearrange("p (a b) -> p a b", a=4): e.tensor_copy(out=o, in_=i_), [st], [wpool])
        k += 1

        TWO_PI = 6.283185307179586
        C1 = 6.28125
        C2 = TWO_PI - C1
        MAGIC = 12582912.0
        PI_LO = 3.1415925
        posf = A.tile("posf", [NSLOT], F32)
        ang = A.tile("ang", [NSLOT, 32], F32)
        kr = A.tile("kr", [NSLOT, 32], F32)
        r1 = A.tile("r1", [NSLOT, 32], F32)
        S.op("dve", lambda e: e.tensor_copy(out=posf.ap, in_=st_pos.ap), [st_pos], [posf])
        S.op("dve", lambda e: e.tensor_tensor(out=ang.ap, in0=posf.ap.unsqueeze(2).to_broadcast([128, NSLOT, 32]),
                                              in1=invf_bc.ap.unsqueeze(1).to_broadcast([128, NSLOT, 32]), op=ALU.mult), [posf, rowb], [ang])
        for which, tab in ((0, sint), (1, cost)):
            off = 0.0 if which == 0 else 0.25
            S.op("dve", lambda e, off=off: e.tensor_scalar(out=kr.ap, in0=ang.ap, scalar1=1.0 / TWO_PI, scalar2=off, op0=ALU.mult, op1=ALU.add), [ang], [kr])
            S.op("dve", lambda e: e.tensor_scalar(out=kr.ap, in0=kr.ap, scalar1=MAGIC, scalar2=None, op0=ALU.add), [kr], [kr])
            S.op("dve", lambda e: e.tensor_scalar(out=kr.ap, in0=kr.ap, scalar1=MAGIC, scalar2=None, op0=ALU.subtract), [kr], [kr])
            S.op("dve", lambda e: e.scalar_tensor_tensor(out=r1.ap, in0=kr.ap, scalar=-C1, in1=ang.ap, op0=ALU.mult, op1=ALU.add), [kr, ang], [r1])
            S.op("dve", lambda e: e.scalar_tensor_tensor(out=r1.ap, in0=kr.ap, scalar=-C2, in1=r1.ap, op0=ALU.mult, op1=ALU.add), [kr, r1], [r1])
            if which == 1:
                S.op("dve", lambda e: e.tensor_scalar(out=r1.ap, in0=r1.ap, scalar1=TWO_PI / 4, scalar2=None, op0=ALU.add), [r1], [r1])
            S.op("dve", lambda e: e.tensor_scalar(out=r1.ap, in0=r1.ap, scalar1=-PI_LO, scalar2=PI_LO, op0=ALU.max, op1=ALU.min), [r1], [r1])
            S.op("act", lambda e, tab=tab: e.activation(out=tab.ap, in_=r1.ap, func=AF.Sin), [r1], [tab])
        dump("sin", sint)
        dump("cos", cost)
        S.stage(1)

        S.barrier()
        A.reset(pmark)

        amark = A.mark()
        It = A.tile("I", [NSLOT * 128], F32, nbufs=8)
        relu = [A.tile(f"relu{i}", [128, 8], F32) for i in range(3)]
        jk = A.tile("jk", [2], F32)
        xn = A.tile("xn", [D], F32)
        hT = A.tile("hT", [8, 128], BF16)
        QQ = A.tile("QQ", [8, 128], BF16)
        QQT = A.tile("QQT", [8, 128], BF16)
        qn = A.tile("qn", [8, 64], F32)
        rt = [A.tile(f"rt{i}", [8, 32], F32) for i in range(2)]
        KK = A.tile("KK", [128], BF16)
        ext = A.tile("ext", [4, 144], F32)
        pA = A.tile("pA", [4, 144], F32)
        pB = A.tile("pB", [4, 144], F32)
        pooledT = A.tile("pooledT", [4, 128], BF16)
        mixin = A.tile("mixin", [D], BF16)
        Mb = [A.tile(f"Mb{i}", [512], BF16) for i in range(2)]
        PT = [A.tile(f"PT{i}", [D], BF16) for i in range(2)]
        h2Tf = A.tile("h2Tf", [8, 128], F32)
        sm = A.tile("sm", [64], F32)
        steps = A.tile("steps", [NIT + 2], F32)
        nsteps = A.tile("nsteps", [NIT + 2], F32)
        tst = A.tile("tst", [2], F32)
        sm2 = A.tile("sm2", [2], F32)
        smo = A.tile("smo", [18], F32)
        wsc = A.tile("wsc", [8], F32)
        lg = A.tile("lg", [36], F32)
        rsm = A.tile("rsm", [96], F32)
        x_ctr = [S.dctr(f"xacc{g}") for g in range(GB)]
        xo_ctr = S.dctr("xo")
        out_ctr = [S.dctr(f"out{g}") for g in range(GB)]
        PT = PT + [T(r.ap.rearrange("p k h -> p (k h)").bitcast(BF16)[:, 0:D], r.bufs) for r in relu[0:2]]
        QQTz = T(relu[2].ap.rearrange("p k h -> p (k h)").bitcast(BF16)[:, 0:D], relu[2].bufs)
        att_end = A.mark()

        smk = [0]

        def col(n=1):
            o = smk[0]
            smk[0] += n
            assert smk[0] <= 64
            return sm.ap[:, o:o + n]

        c_ss = col()
        c_rstd = col()
        c_ssh = col(8)
        c_rh = col(8)
        CS_MAIN = (c_ss, c_rstd, c_ssh, c_rh, sm)
        CS_OTH = (smo.ap[:, 0:1], smo.ap[:, 1:2], smo.ap[:, 2:10], smo.ap[:, 10:18], smo)
        c_B = col()
        c_lo = col()
        c_test = col()
        c_cnt = col()
        c_g = col()
        c_rden = col(8)

        def rms_to_hT(xt, a_idx, dst_bf, dst_f32=None, cs=None):
            c_ss, c_rstd, _, _, sm = cs or CS_MAIN
            if xt is xn:
                S.op("act", lambda e: e.activation(out=jk.ap[:, 0:1].to_broadcast([128, D]), in_=xt.ap, func=AF.Square, accum_out=c_ss), [xt], [jk, sm])
            else:
                S.op("act", lambda e: e.activation(out=xn.ap, in_=xt.ap, func=AF.Square, accum_out=c_ss), [xt], [xn, sm])
            S.op("dve", lambda e: e.tensor_scalar(out=c_rstd, in0=c_ss, scalar1=1.0 / D, scalar2=EPS, op0=ALU.mult, op1=ALU.add), [sm], [sm])
            S.op("act", lambda e: e.activation(out=c_rstd, in_=c_rstd, func=AF.Sqrt), [sm], [sm])
            S.op("dve", lambda e: e.reciprocal(out=c_rstd, in_=c_rstd), [sm], [sm])
            S.op("act", lambda e: e.activation(out=xn.ap, in_=xt.ap, func=AF.Identity, scale=c_rstd), [xt, sm], [xn])
            tp = PS(6, 2)
            tpv = tp.ap.rearrange("p (a b) -> p a b", a=8)
            for c in range(8):
                S.op("pe", lambda e, c=c: e.transpose(tpv[:, c, :], xn.ap[:, c * 128:(c + 1) * 128], ident_f.ap), [xn, ident_f], [tp])
            for c in range(8):
                S.op("act", lambda e, c=c: e.activation(out=dst_bf[0][:, c, :], in_=tpv[:, c, :], func=AF.Identity,
                                                        scale=ab.ap[:, a_idx, c:c + 1], bias=ab.ap[:, a_idx + 1, c:c + 1]), [tp, ab], [dst_bf[1]])
                if dst_f32 is not None:
                    S.op("dve", lambda e, c=c: e.tensor_scalar(out=dst_f32.ap[:, c, :], in0=tpv[:, c, :], scalar1=ab.ap[:, a_idx, c:c + 1],
                                                               scalar2=ab.ap[:, a_idx + 1, c:c + 1], op0=ALU.mult, op1=ALU.add), [tp, ab], [dst_f32])

        def head_norm(src_ps, nh, gbc, dst, cs=None):
            _, _, c_ssh, c_rh, sm = cs or CS_MAIN
            sv = src_ps[0].rearrange("p (h d) -> p h d", h=nh)
            S.op("act", lambda e: e.activation(out=dst.ap[:, 0:nh, :], in_=sv, func=AF.Square), [src_ps[1]], [dst])
            S.op("dve", lambda e: e.tensor_reduce(out=c_ssh[:, 0:nh], in_=dst.ap[:, 0:nh, :], axis=AX.X, op=ALU.add), [dst], [sm])
            S.op("dve", lambda e: e.tensor_scalar(out=c_rh[:, 0:nh], in0=c_ssh[:, 0:nh], scalar1=1.0 / 64, scalar2=EPS, op0=ALU.mult, op1=ALU.add), [sm], [sm])
            S.op("act", lambda e: e.activation(out=c_rh[:, 0:nh], in_=c_rh[:, 0:nh], func=AF.Sqrt), [sm], [sm])
            S.op("dve", lambda e: e.reciprocal(out=c_rh[:, 0:nh], in_=c_rh[:, 0:nh]), [sm], [sm])
            for h in range(nh):
                S.op("dve", lambda e, h=h: e.scalar_tensor_tensor(out=dst.ap[:, h, :], in0=sv[:, h, :], scalar=c_rh[:, h:h + 1],
                                                                  in1=gbc[:, h * 64 % gbc.shape[1]:h * 64 % gbc.shape[1] + 64], op0=ALU.mult, op1=ALU.mult),
                     [src_ps[1], sm, rowb, gq8], [dst])

        def rope(src_ap, src_dep, nh, slot, dst_ap, dst_dep):
            s4 = src_ap.rearrange("p h (two d) -> p h two d", two=2)
            d4 = dst_ap.rearrange("p h (two d) -> p h two d", two=2)
            cb = cost.ap[:, slot, :].unsqueeze(1).to_broadcast([128, nh, 32])
            sb = sint.ap[:, slot, :].unsqueeze(1).to_broadcast([128, nh, 32])
            t0 = rt[0].ap[:, 0:nh, :]
            t1 = rt[1].ap[:, 0:nh, :]
            x1 = s4[:, :, 0, :]
            x2 = s4[:, :, 1, :]
            S.op("dve", lambda e: e.tensor_tensor(out=t0, in0=x1, in1=cb, op=ALU.mult), [src_dep, cost], [rt[0]])
            S.op("dve", lambda e: e.tensor_tensor(out=t1, in0=x2, in1=sb, op=ALU.mult), [src_dep, sint], [rt[1]])
            S.op("dve", lambda e: e.tensor_tensor(out=d4[:, :, 0, :], in0=t0, in1=t1, op=ALU.subtract), [rt[0], rt[1]], [dst_dep])
            S.op("dve", lambda e: e.tensor_tensor(out=t0, in0=x1, in1=sb, op=ALU.mult), [src_dep, sint], [rt[0]])
            S.op("dve", lambda e: e.tensor_tensor(out=t1, in0=x2, in1=cb, op=ALU.mult), [src_dep, cost], [rt[1]])
            S.op("dve", lambda e: e.tensor_tensor(out=d4[:, :, 1, :], in0=t0, in1=t1, op=ALU.add), [rt[0], rt[1]], [dst_dep])

        def kside(slot, kv_ps, cs=None):
            kkb = kk.bufs[slot]
            vb = Vt.bufs[slot]
            head_norm((kv_ps.ap[:, 0:128], kv_ps), 2, gk_bc.ap, qn, cs=cs)
            rope(qn.ap[:, 0:2, :], qn, 2, slot, KK.ap.rearrange("p (h d) -> p h d", h=2), KK)
            S.op("act", lambda e: e.copy(out=Vt.ap[:, slot, 0:64], in_=kv_ps.ap[:, 128:192]), [kv_ps], [vb])
            kt = PS(3)
            ktv = kt.ap.bitcast(BF16)[:, 0:128]
            S.op("pe", lambda e: e.transpose(ktv, KK.ap, ident_b.ap), [KK, ident_b], [kt])
            S.op("act", lambda e: e.copy(out=kk.ap[:, slot * 128:(slot + 1) * 128], in_=ktv), [kt], [kkb])

        def other_block(j):
            so = 2 * j
            S.dma("sp", lambda e, j=j: e.dma_start(out=xn.ap, in_=x_oth[j * 128:(j + 1) * 128, :]), [], [xn], xo_ctr)
            rms_to_hT(xn, 0, (hT.ap, hT), cs=CS_OTH)
            kv_ps = PS(5)
            for c in range(8):
                S.op("pe", lambda e, c=c: e.matmul(kv_ps.ap[:, 0:200], hT.ap[:, c, :], win.ap[:, c, 1024:1224], start=(c == 0), stop=(c == 7), skip_group_check=True), [hT, win], [kv_ps])
            uo_v = kv_ps.ap[:, 256:320].rearrange("p (g t) -> p g t", g=4)
            for gg in range(4):
                for c in range(8):
                    S.op("pe", lambda e, c=c, gg=gg: e.matmul(uo_v[:, gg, :], win.ap[:, c, 1224 + gg * 128:1224 + (gg + 1) * 128], hT.ap[:, c, 112:128],
                                                              start=False, stop=(c == 7), skip_group_check=True), [hT, win], [kv_ps])
            if j == 0:
                S.op("dve", lambda e: e.tensor_scalar(out=tailt.ap, in0=uo_v, scalar1=metat.ap[:, 1:2], scalar2=None, op0=ALU.mult), [kv_ps, metat], [tailt])
            else:
                S.op("act", lambda e: e.copy(out=tailt.ap, in_=uo_v), [kv_ps], [tailt])
            kside(so, kv_ps, cs=CS_OTH)


        out_blocks = []
        pend_router_prev = []
        for j in range(NB):
            g = j % GB
            so = 2 * j
            sw = 2 * j + 1
            if j == 0:
                other_block(0)
            S.stage(2)

            xa = acc[g]
            if j % GB == 0:
                S.dma("sp", lambda e, j=j, xa=xa: e.dma_start(out=xa.ap, in_=x_own[j * 128:(j + 1) * 128, :]), [], [xa], x_ctr[g])
            rms_to_hT(xa, 0, (hT.ap, hT))
            if j == 0:
                dump("hT0", hT)
            q_ps = PS(0)
            qi_ps = PS(1)
            kv_ps = PS(5)
            u_ps = PS(4)
            for c in range(8):
                S.op("pe", lambda e, c=c: e.matmul(q_ps.ap, hT.ap[:, c, :], win.ap[:, c, 0:512], start=(c == 0), stop=(c == 7)), [hT, win], [q_ps])
            for c in range(8):
                S.op("pe", lambda e, c=c: e.matmul(qi_ps.ap, hT.ap[:, c, :], win.ap[:, c, 512:1024], start=(c == 0), stop=(c == 7)), [hT, win], [qi_ps])
            for c in range(8):
                S.op("pe", lambda e, c=c: e.matmul(kv_ps.ap[:, 0:200], hT.ap[:, c, :], win.ap[:, c, 1024:1224], start=(c == 0), stop=(c == 7)), [hT, win], [kv_ps])
            u_v = u_ps.ap.rearrange("p (g t) -> p g t", g=4)
            for gg in range(4):
                for c in range(8):
                    S.op("pe", lambda e, c=c, gg=gg: e.matmul(u_v[:, gg, :], win.ap[:, c, 1224 + gg * 128:1224 + (gg + 1) * 128], hT.ap[:, c, :],
                                                              start=(c == 0 and gg == 0), stop=(c == 7), skip_group_check=True), [hT, win], [u_ps])
            head_norm((q_ps.ap, q_ps), 8, gq8.ap, qn)
            rope(qn.ap, qn, 8, sw, QQ.ap[:, :, 0:64], QQ)
            rope(qi_ps.ap.rearrange("p (h d) -> p h d", h=8), qi_ps, 8, sw, QQ.ap[:, :, 64:128], QQ)
            S.op("dve", lambda e: e.tensor_scalar(out=wsc.ap, in0=kv_ps.ap[:, 192:200], scalar1=float(8 ** -0.5 * 64 ** -0.5), scalar2=None, op0=ALU.mult), [kv_ps], [wsc])
            kside(sw, kv_ps)
            qt = PS(2)
            qtv = qt.ap.bitcast(BF16).rearrange("p (h t) -> p h t", h=8)
            for h in range(8):
                S.op("pe", lambda e, h=h: e.transpose(qtv[:, h, :], QQ.ap[:, h, :], ident_b.ap), [QQ, ident_b], [qt])
            S.op("act", lambda e: e.copy(out=QQT.ap, in_=qtv), [qt], [QQT])
            if j == 0:
                dump("QQT0", QQT)
                dump("kk", T(kk.ap[:, 0:256], kk.bufs[0:2]))
            S.stage(3)
            S.op("act", lambda e: e.copy(out=ext.ap[:, :, 16:144], in_=u_v), [u_ps], [ext])
            S.defer_begin()
            S.op("pool", lambda e: e.tensor_copy(out=ext.ap[:, :, 0:16], in_=tailt.ap), [tailt], [ext])
            S.op("pool", lambda e: e.tensor_tensor(out=pA.ap[:, 0:4, 1:144], in0=ext.ap[:, 0:4, 1:144], in1=ext.ap[:, 0:4, 0:143], op=ALU.add), [ext], [pA])
            S.op("pool", lambda e: e.tensor_tensor(out=pB.ap[:, 1:4, 3:144], in0=pA.ap[:, 1:4, 3:144], in1=pA.ap[:, 1:4, 1:142], op=ALU.add), [pA], [pB])
            S.op("pool", lambda e: e.tensor_tensor(out=pA.ap[:, 2:4, 7:144], in0=pB.ap[:, 2:4, 7:144], in1=pB.ap[:, 2:4, 3:140], op=ALU.add), [pB], [pA])
            S.op("pool", lambda e: e.tensor_tensor(out=pB.ap[:, 3:4, 15:144], in0=pA.ap[:, 3:4, 15:144], in1=pA.ap[:, 3:4, 7:136], op=ALU.add), [pA], [pB])
            for gg, src in ((0, pA), (1, pB), (2, pA), (3, pB)):
                wv = src.ap[:, gg, 16:144]
                if j == 0:
                    S.op("pool", lambda e, wv=wv, gg=gg: e.tensor_tensor(out=wv, in0=wv, in1=rc0.ap[:, gg, :], op=ALU.mult), [src, rc0], [src])
                else:
                    S.op("pool", lambda e, wv=wv, gg=gg: e.tensor_scalar(out=wv, in0=wv, scalar1=1.0 / (2 << gg), scalar2=None, op0=ALU.mult), [src], [src])
                S.op("pool", lambda e, wv=wv, gg=gg: e.tensor_tensor(out=pooledT.ap[:, gg, :], in0=wv, in1=ext.ap[:, gg, 16:144], op=ALU.subtract), [src, ext], [pooledT])
            pm_ps = PS(3)
            for gg in range(4):
                S.op("pe", lambda e, gg=gg: e.matmul(pm_ps.ap[:, gg * 128:(gg + 1) * 128], pooledT.ap[:, gg, :], wpool.ap[:, gg, :], start=(gg == 0), stop=True, skip_group_check=True), [pooledT, wpool], [pm_ps])
            S.op("dve", lambda e: e.tensor_tensor(out=mixin.ap[:, 512:1024], in0=pm_ps.ap, in1=pscale_bc.ap, op=ALU.mult), [pm_ps, rowb], [mixin])
            pend_pool = S.defer_end()

            S.stage(4)
            if (j + 1) % GB != 0 and j + 1 < NB:
                xnx = acc[(j + 1) % GB]
                S.dma("sp", lambda e, j=j, xnx=xnx: e.dma_start(out=xnx.ap, in_=x_own[(j + 1) * 128:(j + 2) * 128, :]), [], [xnx], x_ctr[(j + 1) % GB])
            nslots = 2 * j + 2
            nk = nslots * 128
            for s in range(nslots):
                sp_ = PS((s % 3) * 2, 2)
                spv = sp_.ap.rearrange("p (h k) -> p h k", h=8)
                rl = relu[s % 3]
                for h in range(8):
                    S.op("pe", lambda e, h=h, s=s, spv=spv: e.matmul(spv[:, h, :], QQT.ap[64:128, h, :], kk.ap[64:128, s * 128:(s + 1) * 128],
                                                                     start=(h % 4 == 0), stop=True, skip_group_check=True), [QQT, kk.bufs[s]], [sp_])
                S.op("act", lambda e, rl=rl, spv=spv: e.activation(out=rl.ap, in_=spv.rearrange("p h k -> p k h"), func=AF.Relu), [sp_], [rl])
                S.op("pool" if s % 3 != 2 else "dve", lambda e, rl=rl: e.tensor_tensor(out=rl.ap, in0=rl.ap, in1=wsc.ap.unsqueeze(1).to_broadcast([128, 128, 8]), op=ALU.mult), [rl, wsc], [rl])
                Ic = It.ap[:, s * 128:(s + 1) * 128]
                Ib = It.bufs[s // 8]
                S.op("dve", lambda e, rl=rl, Ic=Ic: e.tensor_reduce(out=Ic, in_=rl.ap, axis=AX.X, op=ALU.add), [rl], [Ib])
            Ibs = It.bufs[0:(nslots + 7) // 8]
            S.op("dve", lambda e, nk=nk: e.tensor_reduce(out=c_B, in_=It.ap[:, 0:nk], axis=AX.X, op=ALU.max, apply_absolute_value=True), Ibs, [sm])
            S.op("dve", lambda e: e.tensor_scalar(out=It.ap[:, 0:128], in0=It.ap[:, 0:128], scalar1=metat.ap[:, 0:1], scalar2=None, op0=ALU.add), [It.bufs[0], metat], [It.bufs[0]])
            lastI = It.ap[:, (nslots - 1) * 128:nslots * 128]
            S.op("dve", lambda e, lastI=lastI: e.tensor_tensor(out=lastI, in0=lastI, in1=tri.ap, op=ALU.add), [It.bufs[(nslots - 1) // 8], tri], [It.bufs[(nslots - 1) // 8]])
            nd_slots = nslots if nslots < 4 else max(1, int(round(nslots * DVE_FRAC)))
            nd = nd_slots * 128
            na = nk - nd
            Ibd = It.bufs[0:(nd_slots + 7) // 8]
            Iba = It.bufs[nd_slots // 8:(nslots + 7) // 8]
            S.op("dve", lambda e: e.tensor_scalar(out=c_B, in0=c_B, scalar1=1.001, scalar2=1e-30, op0=ALU.mult, op1=ALU.add), [sm], [sm])
            S.op("dve", lambda e: e.tensor_scalar(out=steps.ap, in0=pow2_bc.ap, scalar1=c_B, scalar2=None, op0=ALU.mult), [sm, rowb], [steps])
            S.op("dve", lambda e: e.tensor_scalar(out=nsteps.ap, in0=steps.ap, scalar1=-1.0, scalar2=None, op0=ALU.mult), [steps], [nsteps])
            S.op("dve", lambda e: e.tensor_scalar(out=c_lo, in0=c_B, scalar1=-1.0, scalar2=None, op0=ALU.mult), [sm], [sm])
            thr = float(512 - na)
            pend = []
            if j + 1 < NB:
                S.defer_begin()
                other_block(j + 1)
                pend = S.defer_end()
            pend = pend_router_prev + pend_pool + pend
            per_it = (len(pend) + NIT - 1) // NIT
            for it in range(NIT):
                S.op("dve", lambda e, it=it: e.tensor_tensor(out=tst.ap[:, 0:1], in0=c_lo, in1=steps.ap[:, it:it + 1], op=ALU.add), [sm, steps], [tst])
                S.op("dve", lambda e, nd=nd: e.tensor_scalar(out=c_g.to_broadcast([128, nd]), in0=It.ap[:, 0:nd], scalar1=tst.ap[:, 0:1], scalar2=None,
                                                           op0=ALU.is_ge, op1=ALU.add, accum_out=c_cnt), [tst] + Ibd, [sm])
                if na > 0:
                    S.op("act", lambda e, nd=nd, nk=nk, na=na: e.activation(out=sm2.ap[:, 1:2].to_broadcast([128, na]), in_=It.ap[:, nd:nk], func=AF.Sign,
                                                                          bias=tst.ap[:, 0:1], scale=-1.0, accum_out=sm2.ap[:, 0:1]), [tst] + Iba, [sm2])
                    S.emit_some(pend, per_it)
                    S.op("dve", lambda e: e.scalar_tensor_tensor(out=c_cnt, in0=c_cnt, scalar=2.0, in1=sm2.ap[:, 0:1], op0=ALU.mult, op1=ALU.subtract), [sm, sm2], [sm])
                    S.op("dve", lambda e, it=it, thr=thr: e.tensor_scalar(out=c_g, in0=c_cnt, scalar1=thr, scalar2=steps.ap[:, it:it + 1], op0=ALU.is_ge, op1=ALU.mult), [sm, steps], [sm])
                else:
                    S.emit_some(pend, per_it)
                    S.op("dve", lambda e, it=it: e.tensor_scalar(out=c_g, in0=c_cnt, scalar1=256.0, scalar2=steps.ap[:, it:it + 1], op0=ALU.is_ge, op1=ALU.mult), [sm, steps], [sm])
                S.op("dve", lambda e: e.tensor_tensor(out=c_lo, in0=c_lo, in1=c_g, op=ALU.add), [sm], [sm])
            S.emit_some(pend, len(pend))
            if j in (0, 1, 5):
                dump(f"I{j}", T(It.ap[:, 0:nk], Ibs))
                dump(f"lo{j}", sm, ap=c_lo)

            S.stage(5)
            o_ps = PS(4, 2)
            oT = o_ps.ap[0:66, :]

            def emit_m01(s0):
                mbt = Mb[(s0 // 4) % 2]
                ns4 = min(4, nslots - s0)
                S.op("dve", lambda e, mbt=mbt, s0=s0, ns4=ns4: e.tensor_scalar(out=mbt.ap[:, 0:ns4 * 128], in0=It.ap[:, s0 * 128:(s0 + ns4) * 128], scalar1=c_lo, scalar2=None,
                                                                            op0=ALU.is_ge), [sm, It.bufs[s0 // 8]], [mbt])

            def emit_qk(s_):
                lt = PS((s_ % 2) * 2, 2)
                for half in range(2):
                    S.op("pe", lambda e, s_=s_, half=half, lt=lt: e.matmul(lt.ap[:, half * 512:(half + 1) * 512], kk.ap[:, s_ * 128:(s_ + 1) * 128],
                                                                          QQTz.ap[:, half * 512:(half + 1) * 512], start=True, stop=True),
                         [kk.bufs[s_], QQTz], [lt])
                mbt = Mb[(s_ // 4) % 2]
                mt = PS(6 + s_ % 2)
                S.op("pe", lambda e, mbt=mbt, mt=mt, s_=s_: e.transpose(mt.ap.bitcast(BF16)[:, 0:128], mbt.ap[:, (s_ % 4) * 128:(s_ % 4 + 1) * 128], ident_b.ap), [mbt, ident_b], [mt])

            S.op("pool", lambda e: e.memset(QQTz.ap[64:128, :], 0.0), [], [QQTz])
            S.op("pool", lambda e: e.tensor_copy(out=QQTz.ap[0:64, :], in_=QQT.ap[0:64, :, :].rearrange("p h t -> p (h t)")), [QQT], [QQTz])
            emit_m01(0)
            emit_qk(0)
            for s in range(nslots):
                if s + 1 < nslots:
                    if (s + 1) % 4 == 0:
                        emit_m01(s + 1)
                    emit_qk(s + 1)
                lt = PS((s % 2) * 2, 2)
                mt = PS(6 + s % 2)
                pt = PT[s % len(PT)]
                S.op("act", lambda e, pt=pt, lt=lt: e.activation(out=pt.ap, in_=lt.ap, func=AF.Exp), [lt], [pt])
                S.op("dve", lambda e, pt=pt, mt=mt: e.tensor_tensor(out=pt.ap.rearrange("p (h t) -> p h t", h=8), in0=pt.ap.rearrange("p (h t) -> p h t", h=8),
                                                                   in1=mt.ap.bitcast(BF16)[:, 0:128].unsqueeze(1).to_broadcast([128, 8, 128]), op=ALU.mult), [pt, mt], [pt])
                for half in range(2):
                    S.op("pe", lambda e, s=s, half=half, pt=pt: e.matmul(oT[:, half * 512:(half + 1) * 512], Vt.ap[:, s, :], pt.ap[:, half * 512:(half + 1) * 512],
                                                                        start=(s == 0), stop=(s == nslots - 1)), [pt, Vt.bufs[s]], [o_ps])
            oTs = xn.ap[0:66, :]
            S.op("act", lambda e: e.copy(out=oTs, in_=oT), [o_ps], [xn])
            o2 = PS(0, 2)
            o_v = o2.ap.rearrange("p (h d) -> p h d", h=8)
            for h in range(8):
                S.op("pe", lambda e, h=h: e.transpose(o_v[:, h, 0:66], oTs[:, h * 128:(h + 1) * 128], ident_f.ap[0:66, 0:66]), [xn, ident_f], [o2])
            o_ps = o2
            S.op("dve", lambda e: e.reciprocal(out=c_rden, in_=o_v[:, :, 64]), [o_ps], [sm])
            S.op("dve", lambda e: e.tensor_tensor(out=mixin.ap[:, 0:512].rearrange("p (h d) -> p h d", h=8), in0=o_v[:, :, 0:64],
                                                  in1=c_rden.unsqueeze(2).to_broadcast([128, 8, 64]), op=ALU.mult), [o_ps, sm], [mixin])
            if j in (0, 1, 5):
                dump(f"mixin{j}", mixin)

            S.stage(6)
            defer_router = (g != GB - 1)
            if defer_router:
                S.defer_begin()
            mt = PS(2)
            mtv = mt.ap.bitcast(BF16).rearrange("p (c t) -> p c t", c=8)
            for c in range(8):
                S.op("pe", lambda e, c=c: e.transpose(mtv[:, c, :], mixin.ap[:, c * 128:(c + 1) * 128], ident_b.ap), [mixin, ident_b], [mt])
            S.op("act", lambda e: e.copy(out=hT.ap, in_=mtv), [mt], [hT])
            y1 = PS(6, 2)
            for half in range(2):
                for c in range(8):
                    S.op("pe", lambda e, c=c, half=half: e.matmul(y1.ap[:, half * 512:(half + 1) * 512], hT.ap[:, c, :], wout.ap[:, c, half * 512:(half + 1) * 512],
                                                                  start=(c == 0), stop=(c == 7)), [hT, wout], [y1])
            S.op("dve", lambda e: e.tensor_tensor(out=xn.ap, in0=y1.ap, in1=G1.ap, op=ALU.mult), [y1, G1], [xn])
            S.op("pool", lambda e, xa=xa: e.tensor_tensor(out=xa.ap, in0=xa.ap, in1=xn.ap, op=ALU.add), [xa, xn], [xa])
            if j in (0, 1, 5):
                dump(f"x1_{j}", xa)

            S.stage(7)
            h2b = h2T.bufs[g]
            rms_to_hT(xa, 2, (h2T.ap[:, :, g * 128:(g + 1) * 128], h2b), dst_f32=h2Tf, cs=(CS_OTH if defer_router else None))
            lg_ps = PS(2)
            for c in range(8):
                S.op("pe", lambda e, c=c: e.matmul(lg_ps.ap[:, 0:36], h2Tf.ap[:, c, :], wr.ap[:, c, :], start=(c == 0), stop=(c == 7)), [h2Tf, wr], [lg_ps])
            S.op("dve", lambda e: e.tensor_tensor(out=lg.ap, in0=lg_ps.ap[:, 0:36], in1=brt_bc.ap, op=ALU.add), [lg_ps, rowb], [lg])
            R = rsm.ap
            r_gmax, r_ngmax, r_sumg, r_pg = R[:, 0:1], R[:, 1:2], R[:, 2:3], R[:, 3:4]
            r_ohg, r_eg, r_esel, r_m8 = R[:, 4:8], R[:, 8:12], R[:, 16:24], R[:, 24:32]
            r_d, r_ed, r_tp1, r_tp2 = R[:, 32:33], R[:, 33:34], R[:, 34:35], R[:, 35:36]
            r_w1, r_w2, r_t48 = R[:, 40:48], R[:, 48:56], R[:, 56:88]
            dd = ([lg, rsm], [rsm])
            S.op("dve", lambda e: e.tensor_reduce(out=r_gmax, in_=lg.ap[:, 0:4], axis=AX.X, op=ALU.max), *dd)
            S.op("dve", lambda e: e.tensor_scalar(out=r_ohg, in0=lg.ap[:, 0:4], scalar1=r_gmax, scalar2=None, op0=ALU.is_equal), *dd)
            S.op("dve", lambda e: e.tensor_scalar(out=r_ngmax, in0=r_gmax, scalar1=-1.0, scalar2=None, op0=ALU.mult), *dd)
            S.op("act", lambda e: e.activation(out=r_eg, in_=lg.ap[:, 0:4], func=AF.Exp, bias=r_ngmax, scale=1.0, accum_out=r_sumg), *dd)
            S.op("dve", lambda e: e.reciprocal(out=r_pg, in_=r_sumg), *dd)
            S.op("dve", lambda e: e.tensor_tensor(out=r_t48.rearrange("p (g x) -> p g x", g=4), in0=lg.ap[:, 4:36].rearrange("p (g x) -> p g x", g=4),
                                                  in1=r_ohg.unsqueeze(2).to_broadcast([128, 4, 8]), op=ALU.mult), *dd)
            S.op("dve", lambda e: e.tensor_reduce(out=r_esel, in_=r_t48.rearrange("p (g x) -> p x g", g=4), axis=AX.X, op=ALU.add), *dd)
            S.op("dve", lambda e: e.max(out=r_m8, in_=r_esel), *dd)
            S.op("dve", lambda e: e.tensor_tensor(out=r_d, in0=r_m8[:, 1:2], in1=r_m8[:, 0:1], op=ALU.subtract), *dd)
            S.op("act", lambda e: e.activation(out=r_ed, in_=r_d, func=AF.Exp), *dd)
            S.op("dve", lambda e: e.tensor_scalar(out=r_tp1, in0=r_ed, scalar1=1.0, scalar2=None, op0=ALU.add), *dd)
            S.op("dve", lambda e: e.reciprocal(out=r_tp1, in_=r_tp1), *dd)
            S.op("dve", lambda e: e.tensor_tensor(out=r_tp2, in0=r_ed, in1=r_tp1, op=ALU.mult), *dd)
            S.op("dve", lambda e: e.tensor_scalar(out=r_w1, in0=r_esel, scalar1=r_m8[:, 0:1], scalar2=r_tp1, op0=ALU.is_equal, op1=ALU.mult), *dd)
            S.op("dve", lambda e: e.tensor_scalar(out=r_w2, in0=r_esel, scalar1=r_m8[:, 1:2], scalar2=r_tp2, op0=ALU.is_equal, op1=ALU.mult), *dd)
            S.op("dve", lambda e: e.tensor_tensor(out=r_w1, in0=r_w1, in1=r_w2, op=ALU.add), *dd)
            S.op("dve", lambda e: e.tensor_scalar(out=r_ohg, in0=r_ohg, scalar1=r_pg, scalar2=None, op0=ALU.mult), *dd)
            gt_ = gates[g]
            S.op("dve", lambda e, gt_=gt_: e.tensor_tensor(out=gt_.ap.rearrange("p (g x) -> p g x", g=4), in0=r_ohg.unsqueeze(2).to_broadcast([128, 4, 8]),
                                                          in1=r_w1.unsqueeze(1).to_broadcast([128, 4, 8]), op=ALU.mult), [rsm], [gt_])
            if j in (0, 1, 5):
                dump(f"gates{j}", gt_)
            pend_router = S.defer_end() if defer_router else []
            pend_router_prev = pend_router
            out_blocks.append(j)

            S.stage(8)
            if g == GB - 1:
                S.barrier()
                A.reset(att_end if False else amark)
                accy = [A.tile(f"accy{i}", [D], F32) for i in range(GB)]
                sgh = [A.tile(f"sg{i}", [512], F32) for i in range(2)]
                aTh = [A.tile(f"aT{i}", [512], BF16) for i in range(2)]
                stg_g = A.tile("stg_g", [8, DE], F32)
                stg_u = A.tile("stg_u", [8, DE], F32)
                stg_d = A.tile("stg_d", [2, D], F32)
                wgu = [A.tile(f"wgu{i}", [8, 512], BF16) for i in range(2)]
                wd = [A.tile(f"wd{i}", [2, D], BF16) for i in range(2)]
                outst = A.tile("outst", [D], F32)
                tmpo = A.tile("tmpo", [D], F32)
                if j == GB - 1:
                    moe_ctrs = (S.dctr("wg"), S.dctr("wu"), S.dctr("wd"), [S.dctr("wgu0"), S.dctr("wgu1")], [S.dctr("wdb0"), S.dctr("wdb1")], S.dctr("wscr"))
                wg_ctr, wu_ctr, wd_ctr, wgu_ctr, wdb_ctr, wscr_ctr = moe_ctrs
                for ex in range(NE):
                    wgu_ = wgu[ex % 2]
                    wd_ = wd[ex % 2]
                    if j == GB - 1:
                        S.dma("sp", lambda e, ex=ex: e.dma_start(out=stg_g.ap, in_=w_gate[ex].rearrange("(kc p) f -> p kc f", p=128)), [], [stg_g], wg_ctr)
                        S.dma("sp", lambda e, ex=ex: e.dma_start(out=stg_u.ap, in_=w_up[ex].rearrange("(kc p) f -> p kc f", p=128)), [], [stg_u], wu_ctr)
                        S.dma("sp", lambda e, ex=ex: e.dma_start(out=stg_d.ap, in_=w_down[ex].rearrange("(kc p) n -> p kc n", p=128)), [], [stg_d], wd_ctr)
                        S.op("act", lambda e, wgu_=wgu_: e.copy(out=wgu_.ap[:, :, 0:256], in_=stg_g.ap), [stg_g], [wgu_])
                        S.op("pool", lambda e, wgu_=wgu_: e.tensor_copy(out=wgu_.ap[:, :, 256:512], in_=stg_u.ap), [stg_u], [wgu_])
                        S.op("dve", lambda e, wd_=wd_: e.tensor_copy(out=wd_.ap, in_=stg_d.ap), [stg_d], [wd_])
                        S.dma("sp", lambda e, ex=ex, wgu_=wgu_: e.dma_start(out=wgu_s[ex], in_=wgu_.ap.rearrange("p a b -> p (a b)")), [wgu_], [], wscr_ctr)
                        S.dma("sp", lambda e, ex=ex, wd_=wd_: e.dma_start(out=wd_s[ex], in_=wd_.ap.rearrange("p a b -> p (a b)")), [wd_], [], wscr_ctr)
                    else:
                        S.dma("sp", lambda e, ex=ex, wgu_=wgu_: e.dma_start(out=wgu_.ap.rearrange("p a b -> p (a b)"), in_=wgu_s[ex]), [], [wgu_], wgu_ctr[ex % 2])
                        S.dma("sp", lambda e, ex=ex, wd_=wd_: e.dma_start(out=wd_.ap.rearrange("p a b -> p (a b)"), in_=wd_s[ex]), [], [wd_], wdb_ctr[ex % 2])
                    gu = [PS(b) for b in range(4)]
                    for hh in range(2):
                        for oc in (hh, 2 + hh):
                            for c in range(8):
                                S.op("pe", lambda e, oc=oc, c=c, wgu_=wgu_: e.matmul(gu[oc].ap, wgu_.ap[:, c, oc * 128:(oc + 1) * 128], h2T.ap[:, c, :], start=(c == 0), stop=(c == 7)),
                                     [wgu_, h2T], [gu[oc]])
                        S.op("act", lambda e, hh=hh: e.activation(out=sgh[hh].ap, in_=gu[hh].ap, func=AF.Silu), [gu[hh]], [sgh[hh]])
                        S.op("dve", lambda e, hh=hh: e.tensor_tensor(out=aTh[hh].ap, in0=sgh[hh].ap, in1=gu[2 + hh].ap, op=ALU.mult), [sgh[hh], gu[2 + hh]], [aTh[hh]])
                    for pair in range(GB // 2):
                        blks = (2 * pair, 2 * pair + 1)
                        yps = {blk: PS(4 + (blk % 2) * 2, 2) for blk in blks}
                        for c2 in range(2):
                            for blk in blks:
                                for half in range(2):
                                    S.op("pe", lambda e, half=half, c2=c2, blk=blk, yp=yps[blk], wd_=wd_: e.matmul(yp.ap[:, half * 512:(half + 1) * 512], aTh[c2].ap[:, blk * 128:(blk + 1) * 128],
                                                                                                                wd_.ap[:, c2, half * 512:(half + 1) * 512], start=(c2 == 0), stop=(c2 == 1)),
                                         [aTh[c2], wd_], [yps[blk]])
                        for blk in blks:
                            yp = yps[blk]
                            gcol = gates[blk].ap[:, ex:ex + 1]
                            if ex == 0:
                                S.op("dve", lambda e, blk=blk, yp=yp, gcol=gcol: e.tensor_scalar(out=accy[blk].ap, in0=yp.ap, scalar1=gcol, scalar2=None, op0=ALU.mult), [yp, gates[blk]], [accy[blk]])
                            else:
                                S.op("dve", lambda e, blk=blk, yp=yp, gcol=gcol: e.scalar_tensor_tensor(out=accy[blk].ap, in0=yp.ap, scalar=gcol, in1=accy[blk].ap, op0=ALU.mult, op1=ALU.add),
                                     [yp, gates[blk], accy[blk]], [accy[blk]])
                for blk in range(GB):
                    jb = j - (GB - 1) + blk
                    S.op("dve", lambda e, blk=blk: e.tensor_tensor(out=tmpo.ap, in0=accy[blk].ap, in1=G2.ap, op=ALU.mult), [accy[blk], G2], [tmpo])
                    S.op("dve" if blk % 2 == 0 else "pool", lambda e, blk=blk: e.tensor_tensor(out=outst.ap, in0=tmpo.ap, in1=acc[blk].ap, op=ALU.add), [tmpo, acc[blk]], [outst])
                    S.dma("sp", lambda e, jb=jb: e.dma_start(out=out_d[jb * 128:(jb + 1) * 128, :], in_=outst.ap), [outst], [], out_ctr[blk])
                S.barrier()
                A.reset(att_end)

        S.final_wait("sp")

        block = es.enter_context(nc.Block())

        @block.sync
        def _(e):
            S.replay("sp", e)

        @block.tensor
        def _(e):
            S.replay("pe", e)

        @block.scalar
        def _(e):
            S.replay("act", e)

        @block.vector
        def _(e):
            S.replay("dve", e)

        @block.gpsimd
        def _(e):
            S.replay("pool", e)

    return nc


def _prep_inputs(x, c, positions, w_ada, b_ada, g_norm_mix, g_norm_ffn, w_in, g_q, g_k, g_kidx,
                 w_pool, pool_scale, w_out, w_router_group, b_router_group, w_router_expert,
                 b_router_expert, w_gate, w_up, w_down, cores=range(8)):
    f = np.float32
    x = np.asarray(x, f)
    W = np.asarray(w_in[0], f)
    perm = np.concatenate([np.arange(0, 512), np.arange(640, 1152), np.arange(512, 576), np.arange(1152, 1216),
                           np.arange(576, 640), np.arange(1216, 1224), np.arange(1224, 1736)])
    w_in_p = np.ascontiguousarray(W[:, perm])
    inv_freq = (np.float32(10000.0) ** (-np.arange(0, 64, 2, dtype=np.float32) / np.float32(64))).astype(f)
    pow2 = (2.0 ** -np.arange(24)).astype(f)
    rowp = np.zeros((1, 832), f)
    rowp[0, 0:64] = g_q[0]
    rowp[0, 64:128] = g_k[0]
    rowp[0, 128:192] = g_kidx[0]
    rowp[0, 192:704] = pool_scale[0]
    rowp[0, 704:708] = b_router_group[0]
    rowp[0, 708:740] = b_router_expert[0]
    rowp[0, 740:772] = inv_freq
    rowp[0, 772:796] = pow2
    bg = np.concatenate([b_ada[0][2048:3072], b_ada[0][5120:6144]]).astype(f)[None, :]
    w_r = np.ascontiguousarray(np.concatenate([w_router_group[0], w_router_expert[0]], axis=1).astype(f))
    wp = np.ascontiguousarray(np.asarray(w_pool[0], f).transpose(1, 0, 2).reshape(128, 512))
    shared = {
        "rowp": rowp, "bgate": np.ascontiguousarray(bg), "w_ada": np.ascontiguousarray(w_ada[0], f),
        "w_in": w_in_p, "w_out": np.ascontiguousarray(w_out[0], f), "w_pool": wp, "w_r": w_r,
        "w_gate": np.ascontiguousarray(w_gate[0], f), "w_up": np.ascontiguousarray(w_up[0], f),
        "w_down": np.ascontiguousarray(w_down[0], f),
    }
    in_maps = []
    wins = np.array([2, 4, 8, 16], f)
    for core in cores:
        b, p = core // 2, core % 2
        xb = x[b].reshape(64, 128, D)
        pb = np.asarray(positions[b]).reshape(64, 128).astype(np.int32)
        own = xb[p::2]
        pos_own = pb[p::2]
        if p == 1:
            oth = xb[0::2]
            pos_oth = pb[0::2]
        else:
            oth = np.concatenate([np.zeros((1, 128, D), f), xb[1:63:2]], axis=0)
            pos_oth = np.concatenate([np.zeros((1, 128), np.int32), pb[1:63:2]], axis=0)
        pos = np.zeros((128, 64), np.int32)
        pos[:, 0::2] = pos_oth.T
        pos[:, 1::2] = pos_own.T
        colp = np.zeros((128, 72), f)
        colp[:, 0:8] = np.asarray(c[b], f).reshape(8, 128).T
        colp[:, 8:16] = np.asarray(g_norm_mix[0], f).reshape(8, 128).T
        colp[:, 16:24] = np.asarray(g_norm_ffn[0], f).reshape(8, 128).T
        colp[:, 24:72] = np.asarray(b_ada[0], f).reshape(48, 128).T
        meta = np.zeros((128, 2), f)
        meta[:, 0] = 0.0 if p == 1 else NEG
        meta[:, 1] = float(p)
        t = np.arange(128, dtype=f)
        if p == 0:
            cnt = np.minimum(t[None, :] + 1.0, wins[:, None])
        else:
            cnt = np.broadcast_to(wins[:, None], (4, 128))
        rc = np.broadcast_to((1.0 / cnt).astype(f).reshape(1, 512), (128, 512))
        m = dict(shared)
        m.update({
            "x_own": np.ascontiguousarray(own.reshape(-1, D)),
            "x_oth": np.ascontiguousarray(oth.reshape(-1, D)),
            "pos": pos, "colp": colp, "meta": meta, "rc0": np.ascontiguousarray(rc),
        })
        in_maps.append(m)
    return in_maps


_NC_CACHE = {}


def kernel(**inputs):
    if "nc" not in _NC_CACHE:
        _NC_CACHE["nc"] = build_program()
    nc = _NC_CACHE["nc"]
    in_maps = _prep_inputs(**inputs)
    res = run_bass_kernel_spmd(nc, in_maps, core_ids=list(range(8)))
    out = np.zeros((4, 64, 128, D), np.float32)
    for core in range(8):
        b, p = core // 2, core % 2
        out[b, p::2] = np.asarray(res.results[core]["out"]).reshape(NB, 128, D)
    return out.reshape(4, 8192, D)
```

```python
import numpy as np
from contextlib import ExitStack
import concourse.bass as bass
import concourse.mybir as mybir
from concourse.bass_utils import run_bass_kernel_spmd

F32 = mybir.dt.float32
BF16 = mybir.dt.bfloat16
I32 = mybir.dt.int32
U8 = mybir.dt.uint8
AF = mybir.ActivationFunctionType
ALU = mybir.AluOpType
AX = mybir.AxisListType

D = 1024
NB = 32
NSLOT = 64
GB = 4
NIT = 22
DVE_FRAC = 0.45
NE = 32
DE = 256
EPS = 1e-6
NEG = -3.0e38
MNEG = -30000.0
ARENA_KB = 206

ENGS = ("pe", "act", "dve", "pool", "sp")
NO_SELF = ("pe", "sp")

DEBUG = {}
STOP_AT = [99]


class Buf:
    __slots__ = ("name", "w", "rs", "excl")

    def __init__(self, name, excl=False):
        self.name = name
        self.w = None
        self.rs = {}
        self.excl = excl


class Ctr:
    __slots__ = ("sem", "n", "name")

    def __init__(self, sem, name):
        self.sem = sem
        self.n = 0
        self.name = name


class T:
    def __init__(self, ap, bufs):
        self.ap = ap
        self.bufs = bufs if isinstance(bufs, (list, tuple)) else [bufs]


def _bufs(xs):
    out = []
    for x in xs:
        if x is None:
            continue
        if isinstance(x, Buf):
            out.append(x)
        elif isinstance(x, T):
            out.extend(x.bufs)
        else:
            out.extend(_bufs(x))
    return out


class Sched:
    def __init__(self, nc, es):
        self.nc = nc
        self.es = es
        self.q = {e: [] for e in ENGS}
        self.nsem = 0
        self.ectr = {e: self.ctr("eng_" + e) for e in ENGS}
        self.waited = {e: {} for e in ENGS}
        self.dma_ctrs = []
        self.stopped = False
        self.deferred = None

    def stage(self, n):
        if STOP_AT[0] <= n:
            self.stopped = True

    def ctr(self, name):
        self.nsem += 1
        return Ctr(self.es.enter_context(self.nc.semaphore("s_" + name)), name)

    def dctr(self, name):
        c = self.ctr(name)
        self.dma_ctrs.append(c)
        return c

    def _collect(self, eng, rb, wb, extra=()):
        need = {}
        me = self.ectr[eng]
        wd = self.waited[eng]

        def add(c, v):
            if c is me and eng in NO_SELF:
                return
            if wd.get(c, 0) >= v:
                return
            if need.get(c, 0) < v:
                need[c] = v

        for b in rb:
            if b.w is not None:
                add(*b.w)
        for b in wb:
            if b.w is not None:
                add(*b.w)
            for c, v in b.rs.items():
                add(c, v)
        for c, v in extra:
            add(c, v)
        for c, v in need.items():
            wd[c] = v
        return [(c.sem, v) for c, v in need.items()]

    def defer_begin(self):
        self.deferred = []

    def defer_end(self):
        d = self.deferred
        self.deferred = None
        return d

    def emit_some(self, pend, n):
        while pend and n > 0:
            kind, args = pend.pop(0)
            (self.op if kind == "op" else self.dma)(*args)
            n -= 1

    def op(self, eng, fn, reads=(), writes=()):
        if self.stopped:
            return
        if self.deferred is not None:
            self.deferred.append(("op", (eng, fn, reads, writes)))
            return
        rb = _bufs(reads)
        wb = _bufs(writes)
        if any(b.excl for b in rb):
            wb = wb + [b for b in rb if b.excl]
            rb = [b for b in rb if not b.excl]
        waits = self._collect(eng, rb, wb)
        c = self.ectr[eng]
        c.n += 1
        self.q[eng].append((waits, fn, c.sem, 1))
        for b in rb:
            if b.rs.get(c, 0) < c.n:
                b.rs[c] = c.n
        for b in wb:
            b.w = (c, c.n)
            b.rs = {}

    def dma(self, eng, fn, reads, writes, ctr):
        if self.stopped:
            return
        if self.deferred is not None:
            self.deferred.append(("dma", (eng, fn, reads, writes, ctr)))
            return
        rb = _bufs(reads)
        wb = _bufs(writes)
        waits = self._collect(eng, rb, wb)
        ctr.n += 16
        self.q[eng].append((waits, fn, ctr.sem, 16))
        for b in rb:
            if b.rs.get(ctr, 0) < ctr.n:
                b.rs[ctr] = ctr.n
        for b in wb:
            b.w = (ctr, ctr.n)
            b.rs = {}

    def barrier(self):
        if self.stopped:
            return
        snap = [(self.ectr[e], self.ectr[e].n) for e in ENGS if self.ectr[e].n > 0]
        snap += [(c, c.n) for c in self.dma_ctrs if c.n > 0]
        for e in ENGS:
            waits = self._collect(e, [], [], extra=snap)
            if waits:
                self.q[e].append((waits, None, None, 0))

    def final_wait(self, eng="sp"):
        self.stopped = False
        snap = [(self.ectr[e], self.ectr[e].n) for e in ENGS if self.ectr[e].n > 0 and e != eng]
        snap += [(c, c.n) for c in self.dma_ctrs if c.n > 0]
        waits = self._collect(eng, [], [], extra=snap)
        if waits:
            self.q[eng].append((waits, None, None, 0))

    def replay(self, eng, e):
        for waits, fn, sem, inc in self.q[eng]:
            for s, v in waits:
                e.wait_ge(s, v)
            if fn is not None:
                ins = fn(e)
                ins.then_inc(sem, inc)


class Arena:
    def __init__(self, ap_u8, nbytes):
        self.ap = ap_u8
        self.nbytes = nbytes
        self.off = 0

    def mark(self):
        return self.off

    def reset(self, m):
        self.off = m

    def tile(self, name, free_shape, dt, nbufs=None, parts=128):
        esz = {F32: 4, BF16: 2, I32: 4, U8: 1}[dt]
        n = 1
        for s in free_shape:
            n *= s
        nb = n * esz
        self.off = (self.off + 63) // 64 * 64
        assert self.off + nb <= self.nbytes, f"arena overflow at {name}: {self.off + nb} > {self.nbytes}"
        ap = self.ap[:, self.off:self.off + nb]
        self.off += nb
        if dt != U8:
            ap = ap.bitcast(dt)
        if len(free_shape) == 2:
            ap = ap.rearrange("p (a b) -> p a b", a=free_shape[0])
        elif len(free_shape) == 3:
            ap = ap.rearrange("p (a b c) -> p a b c", a=free_shape[0], b=free_shape[1])
        if nbufs is None:
            bufs = [Buf(name)]
        else:
            bufs = [Buf(f"{name}{i}") for i in range(nbufs)]
        return T(ap, bufs)


def build_program(dbg=None):
    dbg = dbg or {}
    nc = bass.Bass("TRN2", target_bir_lowering=False, dynamic_dma_scratch_size=1024)
    dram = {}

    def din(name, shape, dt=F32):
        dram[name] = nc.dram_tensor(name, list(shape), dt, kind="ExternalInput").ap()
        return dram[name]

    x_own = din("x_own", [NB * 128, D])
    x_oth = din("x_oth", [NB * 128, D])
    pos_in = din("pos", [128, NSLOT], I32)
    colp = din("colp", [128, 72])
    rowp = din("rowp", [1, 832])
    bgate = din("bgate", [1, 2048])
    meta = din("meta", [128, 2])
    rc0_in = din("rc0", [128, 4 * 128])
    w_ada = din("w_ada", [D, 6 * D])
    w_in = din("w_in", [D, 1736])
    w_out = din("w_out", [D, D])
    w_pool = din("w_pool", [128, 4 * 128])
    w_r = din("w_r", [D, 36])
    w_gate = din("w_gate", [NE, D, DE])
    w_up = din("w_up", [NE, D, DE])
    w_down = din("w_down", [NE, DE, D])
    out_d = nc.dram_tensor("out", [NB * 128, D], F32, kind="ExternalOutput").ap()
    wgu_s = nc.dram_tensor("wgu_s", [NE, 128, 8 * 512], BF16, kind="Internal").ap()
    wd_s = nc.dram_tensor("wd_s", [NE, 128, 2 * D], BF16, kind="Internal").ap()
    dbg_d = {}
    for k, (shape, dt) in dbg.items():
        dbg_d[k] = nc.dram_tensor("dbg_" + k, list(shape), dt, kind="ExternalOutput").ap()

    with ExitStack() as es:
        arena_t = es.enter_context(nc.sbuf_tensor("arena", [128, ARENA_KB * 1024], U8))
        ps_t = es.enter_context(nc.psum_tensor("ps", [128, 8, 512], F32))
        S = Sched(nc, es)
        A = Arena(arena_t[:, :], ARENA_KB * 1024)
        pbank = [Buf(f"bank{i}", excl=True) for i in range(8)]

        def PS(b0, nb=1):
            ap = ps_t[:, b0:b0 + nb, :].rearrange("p a b -> p (a b)") if nb > 1 else ps_t[:, b0, :]
            return T(ap, pbank[b0:b0 + nb])

        dbg_ctr = S.dctr("dbg") if dbg else None

        def dump(name, t, ap=None):
            if name not in dbg_d:
                return
            src = ap if ap is not None else t.ap
            S.dma("sp", lambda e, o=dbg_d[name], i=src: e.dma_start(out=o, in_=i), [t], [], dbg_ctr)

        ident_f = A.tile("ident_f", [128], F32)
        ident_b = A.tile("ident_b", [128], BF16)
        irep = A.tile("irep", [4, 128], BF16)
        tri = A.tile("tri", [128], F32)
        ab = A.tile("ab", [4, 8], F32)
        G1 = A.tile("G1", [D], F32)
        G2 = A.tile("G2", [D], F32)
        rowb = A.tile("rowb", [832], F32)
        gq8 = A.tile("gq8", [64], F32)
        metat = A.tile("metat", [2], F32)
        rc0 = A.tile("rc0", [4, 128], F32)
        win = A.tile("win", [8, 1736], BF16)
        wout = A.tile("wout", [8, D], BF16)
        wpool = A.tile("wpool", [4, 128], BF16)
        wr = A.tile("wr", [8, 36], F32)
        kk = A.tile("kk", [NSLOT * 128], BF16, nbufs=NSLOT)
        Vt = A.tile("V", [NSLOT, 66], BF16, nbufs=NSLOT)
        sint = A.tile("sin", [NSLOT, 32], F32)
        cost = A.tile("cos", [NSLOT, 32], F32)
        acc = [A.tile(f"acc{g}", [D], F32) for g in range(GB)]
        h2T = A.tile("h2T", [8, GB * 128], BF16, nbufs=GB)
        gates = [A.tile(f"gates{g}", [NE], F32) for g in range(GB)]
        tailt = A.tile("tailt", [4, 16], F32)
        pmark = A.mark()
        cols = A.tile("cols", [72], F32)
        modc = A.tile("modc", [48], F32)
        sc2 = A.tile("sc2", [8, 2], F32)

        gk_bc = T(rowb.ap[:, 64:192], rowb.bufs)
        pscale_bc = T(rowb.ap[:, 192:704], rowb.bufs)
        brt_bc = T(rowb.ap[:, 704:740], rowb.bufs)
        invf_bc = T(rowb.ap[:, 740:772], rowb.bufs)
        pow2_bc = T(rowb.ap[:, 772:796], rowb.bufs)

        setup_ctr = S.dctr("setup")

        def ld(t, src, ap=None, ctr=None):
            dst = ap if ap is not None else t.ap
            S.dma("sp", lambda e, o=dst, i=src: e.dma_start(out=o, in_=i), [], [t], ctr or setup_ctr)

        ld(cols, colp)
        ld(rowb, rowp.partition_broadcast(128).rearrange("p a b -> p (a b)"))
        ld(metat, meta)
        ld(rc0, rc0_in.rearrange("p (a b) -> p a b", a=4))
        ld(wr, w_r.rearrange("(kc p) n -> p kc n", p=128))
        st_pos = A.tile("st_pos", [NSLOT], I32)
        ld(st_pos, pos_in)
        st_bg = A.tile("st_bg", [2048], F32, parts=1)
        ld(st_bg, bgate, ap=st_bg.ap[0:1, :])
        fin = (setup_ctr, setup_ctr.n)
        for t in (cols, rowb, metat, rc0, wr, st_pos, st_bg):
            t.bufs[0].w = fin

        io_i = A.tile("io_i", [128], I32)
        io_f = A.tile("io_f", [128], F32)
        S.op("pool", lambda e: e.iota(io_i.ap, pattern=[[1, 128]], base=0, channel_multiplier=-1), [], [io_i])
        S.op("dve", lambda e: e.tensor_copy(out=io_f.ap, in_=io_i.ap), [io_i], [io_f])
        S.op("dve", lambda e: e.tensor_scalar(out=ident_f.ap, in0=io_f.ap, scalar1=0.0, scalar2=None, op0=ALU.is_equal), [io_f], [ident_f])
        S.op("dve", lambda e: e.tensor_copy(out=ident_b.ap, in_=ident_f.ap), [ident_f], [ident_b])
        S.op("dve", lambda e: e.tensor_copy(out=irep.ap, in_=ident_f.ap.unsqueeze(1).to_broadcast([128, 4, 128])), [ident_f], [irep])
        S.op("dve", lambda e: e.tensor_scalar(out=tri.ap, in0=io_f.ap, scalar1=0.0, scalar2=NEG, op0=ALU.is_gt, op1=ALU.mult), [io_f], [tri])
        ones1 = A.tile("ones1", [128], F32, parts=1)
        S.op("dve", lambda e: e.memset(ones1.ap[0:1, :], 1.0), [], [ones1])
        S.op("dve", lambda e: e.memset(Vt.ap[:, :, 64:65], 1.0), [], [Vt])
        S.op("dve", lambda e: e.memset(Vt.ap[:, :, 65:66], 0.0), [], [Vt])
        S.op("dve", lambda e: e.tensor_scalar(out=gq8.ap, in0=rowb.ap[:, 0:64], scalar1=0.125, scalar2=None, op0=ALU.mult), [rowb], [gq8])

        S.op("act", lambda e: e.activation(out=sc2.ap[:, :, 0], in_=cols.ap[:, 0:8], func=AF.Silu), [cols], [sc2])
        S.op("act", lambda e: e.activation(out=sc2.ap[:, :, 1], in_=cols.ap[:, 0:8], func=AF.Silu), [cols], [sc2])
        scb = A.tile("scb", [8, 128], F32)
        S.op("dve", lambda e: e.tensor_copy(out=scb.ap, in_=sc2.ap[:, :, 0:1].to_broadcast([128, 8, 128])), [sc2], [scb])

        stg = [A.tile(f"stg{i}", [8, 512], F32) for i in range(2)]
        stg_ctr = [S.dctr(f"stg{i}") for i in range(2)]
        wada_v = w_ada.rearrange("(kc p) n -> p kc n", p=128)
        mod_ps = PS(0)
        mod_v = mod_ps.ap[:, 0:96].rearrange("p (a b) -> p a b", b=2)
        first_mod = True
        for i in range(12):
            st = stg[i % 2]
            S.dma("sp", lambda e, o=st.ap, s=wada_v[:, :, i * 512:(i + 1) * 512]: e.dma_start(out=o, in_=s), [], [st], stg_ctr[i % 2])
            for sub in range(4):
                col = i * 4 + sub
                for kc in range(8):
                    S.op("pe", lambda e, o=mod_v[:, col, :], l=st.ap[:, kc, sub * 128:(sub + 1) * 128], r=sc2.ap[:, kc, :], f=first_mod, kc=kc:
                         e.matmul(o, l, r, start=f, stop=(kc == 7), skip_group_check=True), [st, sc2], [mod_ps])
                    first_mod = False
            if i in (4, 5, 10, 11):
                gt = G1 if i < 6 else G2
                half = i % 2
                boff = (0 if i < 6 else 1024) + half * 512
                gps = PS(1 + (i % 2))
                for kc in range(8):
                    S.op("pe", lambda e, o=gps.ap, l=scb.ap[:, kc, :], r=st.ap[:, kc, :], kc=kc:
                         e.matmul(o, l, r, start=(kc == 0), stop=False), [st, scb], [gps])
                S.op("pe", lambda e, o=gps.ap, l=ones1.ap[0:1, :], r=st_bg.ap[0:1, boff:boff + 512]:
                     e.matmul(o, l, r, start=False, stop=True), [ones1, st_bg], [gps])
                S.op("act", lambda e, o=gt.ap[:, half * 512:(half + 1) * 512], i_=gps.ap: e.copy(out=o, in_=i_), [gps], [gt])
        S.op("dve", lambda e: e.tensor_tensor(out=modc.ap, in0=mod_v[:, :, 0], in1=cols.ap[:, 24:72], op=ALU.add), [mod_ps, cols], [modc])
        S.op("dve", lambda e: e.scalar_tensor_tensor(out=ab.ap[:, 0, :], in0=modc.ap[:, 8:16], scalar=1.0, in1=cols.ap[:, 8:16], op0=ALU.add, op1=ALU.mult), [modc, cols], [ab])
        S.op("dve", lambda e: e.tensor_copy(out=ab.ap[:, 1, :], in_=modc.ap[:, 0:8]), [modc], [ab])
        S.op("dve", lambda e: e.scalar_tensor_tensor(out=ab.ap[:, 2, :], in0=modc.ap[:, 32:40], scalar=1.0, in1=cols.ap[:, 16:24], op0=ALU.add, op1=ALU.mult), [modc, cols], [ab])
        S.op("dve", lambda e: e.tensor_copy(out=ab.ap[:, 3, :], in_=modc.ap[:, 24:32]), [modc], [ab])
        dump("ab", ab)
        dump("G1", G1)

        win_v = w_in.rearrange("(kc p) n -> p kc n", p=128)
        k = 0
        for (c0, c1) in ((0, 512), (512, 1024), (1024, 1536), (1536, 1736)):
            st = stg[k % 2]
            S.dma("sp", lambda e, o=st.ap[:, :, 0:c1 - c0], s=win_v[:, :, c0:c1]: e.dma_start(out=o, in_=s), [], [st], stg_ctr[k % 2])
            S.op("pool", lambda e, o=win.ap[:, :, c0:c1], i_=st.ap[:, :, 0:c1 - c0]: e.tensor_copy(out=o, in_=i_), [st], [win])
            k += 1
        wout_v = w_out.rearrange("(kc p) n -> p kc n", p=128)
        for (c0, c1) in ((0, 512), (512, 1024)):
            st = stg[k % 2]
            S.dma("sp", lambda e, o=st.ap, s=wout_v[:, :, c0:c1]: e.dma_start(out=o, in_=s), [], [st], stg_ctr[k % 2])
            S.op("pool", lambda e, o=wout.ap[:, :, c0:c1], i_=st.ap: e.tensor_copy(out=o, in_=i_), [st], [wout])
            k += 1
        st = stg[k % 2]
        S.dma("sp", lambda e, o=st.ap[:, 0, :], s=w_pool: e.dma_start(out=o, in_=s), [], [st], stg_ctr[k % 2])
        S.op("pool", lambda e, o=wpool.ap, i_=st.ap[:, 0, :].rearrange("p (a b) -> p a b", a=4): e.tensor_copy(out=o, in_=i_), [st], [wpool])
        k += 1

        TWO_PI = 6.283185307179586
        C1 = 6.28125
        C2 = TWO_PI - C1
        MAGIC = 12582912.0
        PI_LO = 3.1415925
        posf = A.tile("posf", [NSLOT], F32)
        ang = A.tile("ang", [NSLOT, 32], F32)
        kr = A.tile("kr", [NSLOT, 32], F32)
        r1 = A.tile("r1", [NSLOT, 32], F32)
        S.op("dve", lambda e: e.tensor_copy(out=posf.ap, in_=st_pos.ap), [st_pos], [posf])
        S.op("dve", lambda e: e.tensor_tensor(out=ang.ap, in0=posf.ap.unsqueeze(2).to_broadcast([128, NSLOT, 32]),
                                              in1=invf_bc.ap.unsqueeze(1).to_broadcast([128, NSLOT, 32]), op=ALU.mult), [posf, rowb], [ang])
        for which, tab in ((0, sint), (1, cost)):
            off = 0.0 if which == 0 else 0.25
            S.op("dve", lambda e, off=off: e.tensor_scalar(out=kr.ap, in0=ang.ap, scalar1=1.0 / TWO_PI, scalar2=off, op0=ALU.mult, op1=ALU.add), [ang], [kr])
            S.op("dve", lambda e: e.tensor_scalar(out=kr.ap, in0=kr.ap, scalar1=MAGIC, scalar2=None, op0=ALU.add), [kr], [kr])
            S.op("dve", lambda e: e.tensor_scalar(out=kr.ap, in0=kr.ap, scalar1=MAGIC, scalar2=None, op0=ALU.subtract), [kr], [kr])
            S.op("dve", lambda e: e.scalar_tensor_tensor(out=r1.ap, in0=kr.ap, scalar=-C1, in1=ang.ap, op0=ALU.mult, op1=ALU.add), [kr, ang], [r1])
            S.op("dve", lambda e: e.scalar_tensor_tensor(out=r1.ap, in0=kr.ap, scalar=-C2, in1=r1.ap, op0=ALU.mult, op1=ALU.add), [kr, r1], [r1])
            if which == 1:
                S.op("dve", lambda e: e.tensor_scalar(out=r1.ap, in0=r1.ap, scalar1=TWO_PI / 4, scalar2=None, op0=ALU.add), [r1], [r1])
            S.op("dve", lambda e: e.tensor_scalar(out=r1.ap, in0=r1.ap, scalar1=-PI_LO, scalar2=PI_LO, op0=ALU.max, op1=ALU.min), [r1], [r1])
            S.op("act", lambda e, tab=tab: e.activation(out=tab.ap, in_=r1.ap, func=AF.Sin), [r1], [tab])
        dump("sin", sint)
        dump("cos", cost)
        S.stage(1)

        S.barrier()
        A.reset(pmark)

        amark = A.mark()
        It = A.tile("I", [NSLOT * 128], F32, nbufs=8)
        relu = [A.tile(f"relu{i}", [128, 8], F32) for i in range(3)]
        jk = A.tile("jk", [2], F32)
        xn = A.tile("xn", [D], F32)
        hT = A.tile("hT", [8, 128], BF16)
        QQ = A.tile("QQ", [8, 128], BF16)
        QQT = A.tile("QQT", [8, 128], BF16)
        qn = A.tile("qn", [8, 64], F32)
        rt = [A.tile(f"rt{i}", [8, 32], F32) for i in range(2)]
        KK = A.tile("KK", [128], BF16)
        ext = A.tile("ext", [4, 144], F32)
        pA = A.tile("pA", [4, 144], F32)
        pB = A.tile("pB", [4, 144], F32)
        pooledT = A.tile("pooledT", [4, 128], BF16)
        mixin = A.tile("mixin", [D], BF16)
        Mb = [A.tile(f"Mb{i}", [512], BF16) for i in range(2)]
        PT = [A.tile(f"PT{i}", [D], BF16) for i in range(2)]
        h2Tf = A.tile("h2Tf", [8, 128], F32)
        sm = A.tile("sm", [64], F32)
        steps = A.tile("steps", [NIT + 2], F32)
        nsteps = A.tile("nsteps", [NIT + 2], F32)
        tst = A.tile("tst", [2], F32)
        sm2 = A.tile("sm2", [2], F32)
        smo = A.tile("smo", [18], F32)
        wsc = A.tile("wsc", [8], F32)
        lg = A.tile("lg", [36], F32)
        rsm = A.tile("rsm", [96], F32)
        x_ctr = [S.dctr(f"xacc{g}") for g in range(GB)]
        xo_ctr = S.dctr("xo")
        out_ctr = [S.dctr(f"out{g}") for g in range(GB)]
        PT = PT + [T(r.ap.rearrange("p k h -> p (k h)").bitcast(BF16)[:, 0:D], r.bufs) for r in relu[0:2]]
        QQTz = T(relu[2].ap.rearrange("p k h -> p (k h)").bitcast(BF16)[:, 0:D], relu[2].bufs)
        att_end = A.mark()

        smk = [0]

        def col(n=1):
            o = smk[0]
            smk[0] += n
            assert smk[0] <= 64
            return sm.ap[:, o:o + n]

        c_ss = col()
        c_rstd = col()
        c_ssh = col(8)
        c_rh = col(8)
        CS_MAIN = (c_ss, c_rstd, c_ssh, c_rh, sm)
        CS_OTH = (smo.ap[:, 0:1], smo.ap[:, 1:2], smo.ap[:, 2:10], smo.ap[:, 10:18], smo)
        c_B = col()
        c_lo = col()
        c_test = col()
        c_cnt = col()
        c_g = col()
        c_rden = col(8)

        def rms_to_hT(xt, a_idx, dst_bf, dst_f32=None, cs=None):
            c_ss, c_rstd, _, _, sm = cs or CS_MAIN
            if xt is xn:
                S.op("act", lambda e: e.activation(out=jk.ap[:, 0:1].to_broadcast([128, D]), in_=xt.ap, func=AF.Square, accum_out=c_ss), [xt], [jk, sm])
            else:
                S.op("act", lambda e: e.activation(out=xn.ap, in_=xt.ap, func=AF.Square, accum_out=c_ss), [xt], [xn, sm])
            S.op("dve", lambda e: e.tensor_scalar(out=c_rstd, in0=c_ss, scalar1=1.0 / D, scalar2=EPS, op0=ALU.mult, op1=ALU.add), [sm], [sm])
            S.op("act", lambda e: e.activation(out=c_rstd, in_=c_rstd, func=AF.Sqrt), [sm], [sm])
            S.op("dve", lambda e: e.reciprocal(out=c_rstd, in_=c_rstd), [sm], [sm])
            S.op("act", lambda e: e.activation(out=xn.ap, in_=xt.ap, func=AF.Identity, scale=c_rstd), [xt, sm], [xn])
            tp = PS(6, 2)
            tpv = tp.ap.rearrange("p (a b) -> p a b", a=8)
            for c in range(8):
                S.op("pe", lambda e, c=c: e.transpose(tpv[:, c, :], xn.ap[:, c * 128:(c + 1) * 128], ident_f.ap), [xn, ident_f], [tp])
            for c in range(8):
                S.op("act", lambda e, c=c: e.activation(out=dst_bf[0][:, c, :], in_=tpv[:, c, :], func=AF.Identity,
                                                        scale=ab.ap[:, a_idx, c:c + 1], bias=ab.ap[:, a_idx + 1, c:c + 1]), [tp, ab], [dst_bf[1]])
                if dst_f32 is not None:
                    S.op("dve", lambda e, c=c: e.tensor_scalar(out=dst_f32.ap[:, c, :], in0=tpv[:, c, :], scalar1=ab.ap[:, a_idx, c:c + 1],
                                                               scalar2=ab.ap[:, a_idx + 1, c:c + 1], op0=ALU.mult, op1=ALU.add), [tp, ab], [dst_f32])

        def head_norm(src_ps, nh, gbc, dst, cs=None):
            _, _, c_ssh, c_rh, sm = cs or CS_MAIN
            sv = src_ps[0].rearrange("p (h d) -> p h d", h=nh)
            S.op("act", lambda e: e.activation(out=dst.ap[:, 0:nh, :], in_=sv, func=AF.Square), [src_ps[1]], [dst])
            S.op("dve", lambda e: e.tensor_reduce(out=c_ssh[:, 0:nh], in_=dst.ap[:, 0:nh, :], axis=AX.X, op=ALU.add), [dst], [sm])
            S.op("dve", lambda e: e.tensor_scalar(out=c_rh[:, 0:nh], in0=c_ssh[:, 0:nh], scalar1=1.0 / 64, scalar2=EPS, op0=ALU.mult, op1=ALU.add), [sm], [sm])
            S.op("act", lambda e: e.activation(out=c_rh[:, 0:nh], in_=c_rh[:, 0:nh], func=AF.Sqrt), [sm], [sm])
            S.op("dve", lambda e: e.reciprocal(out=c_rh[:, 0:nh], in_=c_rh[:, 0:nh]), [sm], [sm])
            for h in range(nh):
                S.op("dve", lambda e, h=h: e.scalar_tensor_tensor(out=dst.ap[:, h, :], in0=sv[:, h, :], scalar=c_rh[:, h:h + 1],
                                                                  in1=gbc[:, h * 64 % gbc.shape[1]:h * 64 % gbc.shape[1] + 64], op0=ALU.mult, op1=ALU.mult),
                     [src_ps[1], sm, rowb, gq8], [dst])

        def rope(src_ap, src_dep, nh, slot, dst_ap, dst_dep):
            s4 = src_ap.rearrange("p h (two d) -> p h two d", two=2)
            d4 = dst_ap.rearrange("p h (two d) -> p h two d", two=2)
            cb = cost.ap[:, slot, :].unsqueeze(1).to_broadcast([128, nh, 32])
            sb = sint.ap[:, slot, :].unsqueeze(1).to_broadcast([128, nh, 32])
            t0 = rt[0].ap[:, 0:nh, :]
            t1 = rt[1].ap[:, 0:nh, :]
            x1 = s4[:, :, 0, :]
            x2 = s4[:, :, 1, :]
            S.op("dve", lambda e: e.tensor_tensor(out=t0, in0=x1, in1=cb, op=ALU.mult), [src_dep, cost], [rt[0]])
            S.op("dve", lambda e: e.tensor_tensor(out=t1, in0=x2, in1=sb, op=ALU.mult), [src_dep, sint], [rt[1]])
            S.op("dve", lambda e: e.tensor_tensor(out=d4[:, :, 0, :], in0=t0, in1=t1, op=ALU.subtract), [rt[0], rt[1]], [dst_dep])
            S.op("dve", lambda e: e.tensor_tensor(out=t0, in0=x1, in1=sb, op=ALU.mult), [src_dep, sint], [rt[0]])
            S.op("dve", lambda e: e.tensor_tensor(out=t1, in0=x2, in1=cb, op=ALU.mult), [src_dep, cost], [rt[1]])
            S.op("dve", lambda e: e.tensor_tensor(out=d4[:, :, 1, :], in0=t0, in1=t1, op=ALU.add), [rt[0], rt[1]], [dst_dep])

        def kside(slot, kv_ps, cs=None):
            kkb = kk.bufs[slot]
            vb = Vt.bufs[slot]
            head_norm((kv_ps.ap[:, 0:128], kv_ps), 2, gk_bc.ap, qn, cs=cs)
            rope(qn.ap[:, 0:2, :], qn, 2, slot, KK.ap.rearrange("p (h d) -> p h d", h=2), KK)
            S.op("act", lambda e: e.copy(out=Vt.ap[:, slot, 0:64], in_=kv_ps.ap[:, 128:192]), [kv_ps], [vb])
            kt = PS(3)
            ktv = kt.ap.bitcast(BF16)[:, 0:128]
            S.op("pe", lambda e: e.transpose(ktv, KK.ap, ident_b.ap), [KK, ident_b], [kt])
            S.op("act", lambda e: e.copy(out=kk.ap[:, slot * 128:(slot + 1) * 128], in_=ktv), [kt], [kkb])

        def other_block(j):
            so = 2 * j
            S.dma("sp", lambda e, j=j: e.dma_start(out=xn.ap, in_=x_oth[j * 128:(j + 1) * 128, :]), [], [xn], xo_ctr)
            rms_to_hT(xn, 0, (hT.ap, hT), cs=CS_OTH)
            kv_ps = PS(5)
            for c in range(8):
                S.op("pe", lambda e, c=c: e.matmul(kv_ps.ap[:, 0:200], hT.ap[:, c, :], win.ap[:, c, 1024:1224], start=(c == 0), stop=(c == 7), skip_group_check=True), [hT, win], [kv_ps])
            uo_v = kv_ps.ap[:, 256:320].rearrange("p (g t) -> p g t", g=4)
            for gg in range(4):
                for c in range(8):
                    S.op("pe", lambda e, c=c, gg=gg: e.matmul(uo_v[:, gg, :], win.ap[:, c, 1224 + gg * 128:1224 + (gg + 1) * 128], hT.ap[:, c, 112:128],
                                                              start=False, stop=(c == 7), skip_group_check=True), [hT, win], [kv_ps])
            if j == 0:
                S.op("dve", lambda e: e.tensor_scalar(out=tailt.ap, in0=uo_v, scalar1=metat.ap[:, 1:2], scalar2=None, op0=ALU.mult), [kv_ps, metat], [tailt])
            else:
                S.op("act", lambda e: e.copy(out=tailt.ap, in_=uo_v), [kv_ps], [tailt])
            kside(so, kv_ps, cs=CS_OTH)


        out_blocks = []
        pend_router_prev = []
        for j in range(NB):
            g = j % GB
            so = 2 * j
            sw = 2 * j + 1
            if j == 0:
                other_block(0)
            S.stage(2)

            xa = acc[g]
            if j % GB == 0:
                S.dma("sp", lambda e, j=j, xa=xa: e.dma_start(out=xa.ap, in_=x_own[j * 128:(j + 1) * 128, :]), [], [xa], x_ctr[g])
            rms_to_hT(xa, 0, (hT.ap, hT))
            if j == 0:
                dump("hT0", hT)
            q_ps = PS(0)
            qi_ps = PS(1)
            kv_ps = PS(5)
            u_ps = PS(4)
            for c in range(8):
                S.op("pe", lambda e, c=c: e.matmul(q_ps.ap, hT.ap[:, c, :], win.ap[:, c, 0:512], start=(c == 0), stop=(c == 7)), [hT, win], [q_ps])
            for c in range(8):
                S.op("pe", lambda e, c=c: e.matmul(qi_ps.ap, hT.ap[:, c, :], win.ap[:, c, 512:1024], start=(c == 0), stop=(c == 7)), [hT, win], [qi_ps])
            for c in range(8):
                S.op("pe", lambda e, c=c: e.matmul(kv_ps.ap[:, 0:200], hT.ap[:, c, :], win.ap[:, c, 1024:1224], start=(c == 0), stop=(c == 7)), [hT, win], [kv_ps])
            u_v = u_ps.ap.rearrange("p (g t) -> p g t", g=4)
            for gg in range(4):
                for c in range(8):
                    S.op("pe", lambda e, c=c, gg=gg: e.matmul(u_v[:, gg, :], win.ap[:, c, 1224 + gg * 128:1224 + (gg + 1) * 128], hT.ap[:, c, :],
                                                              start=(c == 0 and gg == 0), stop=(c == 7), skip_group_check=True), [hT, win], [u_ps])
            head_norm((q_ps.ap, q_ps), 8, gq8.ap, qn)
            rope(qn.ap, qn, 8, sw, QQ.ap[:, :, 0:64], QQ)
            rope(qi_ps.ap.rearrange("p (h d) -> p h d", h=8), qi_ps, 8, sw, QQ.ap[:, :, 64:128], QQ)
            S.op("dve", lambda e: e.tensor_scalar(out=wsc.ap, in0=kv_ps.ap[:, 192:200], scalar1=float(8 ** -0.5 * 64 ** -0.5), scalar2=None, op0=ALU.mult), [kv_ps], [wsc])
            kside(sw, kv_ps)
            qt = PS(2)
            qtv = qt.ap.bitcast(BF16).rearrange("p (h t) -> p h t", h=8)
            for h in range(8):
                S.op("pe", lambda e, h=h: e.transpose(qtv[:, h, :], QQ.ap[:, h, :], ident_b.ap), [QQ, ident_b], [qt])
            S.op("act", lambda e: e.copy(out=QQT.ap, in_=qtv), [qt], [QQT])
            if j == 0:
                dump("QQT0", QQT)
                dump("kk", T(kk.ap[:, 0:256], kk.bufs[0:2]))
            S.stage(3)
            S.op("act", lambda e: e.copy(out=ext.ap[:, :, 16:144], in_=u_v), [u_ps], [ext])
            S.defer_begin()
            S.op("pool", lambda e: e.tensor_copy(out=ext.ap[:, :, 0:16], in_=tailt.ap), [tailt], [ext])
            S.op("pool", lambda e: e.tensor_tensor(out=pA.ap[:, 0:4, 1:144], in0=ext.ap[:, 0:4, 1:144], in1=ext.ap[:, 0:4, 0:143], op=ALU.add), [ext], [pA])
            S.op("pool", lambda e: e.tensor_tensor(out=pB.ap[:, 1:4, 3:144], in0=pA.ap[:, 1:4, 3:144], in1=pA.ap[:, 1:4, 1:142], op=ALU.add), [pA], [pB])
            S.op("pool", lambda e: e.tensor_tensor(out=pA.ap[:, 2:4, 7:144], in0=pB.ap[:, 2:4, 7:144], in1=pB.ap[:, 2:4, 3:140], op=ALU.add), [pB], [pA])
            S.op("pool", lambda e: e.tensor_tensor(out=pB.ap[:, 3:4, 15:144], in0=pA.ap[:, 3:4, 15:144], in1=pA.ap[:, 3:4, 7:136], op=ALU.add), [pA], [pB])
            for gg, src in ((0, pA), (1, pB), (2, pA), (3, pB)):
                wv = src.ap[:, gg, 16:144]
                if j == 0:
                    S.op("pool", lambda e, wv=wv, gg=gg: e.tensor_tensor(out=wv, in0=wv, in1=rc0.ap[:, gg, :], op=ALU.mult), [src, rc0], [src])
                else:
                    S.op("pool", lambda e, wv=wv, gg=gg: e.tensor_scalar(out=wv, in0=wv, scalar1=1.0 / (2 << gg), scalar2=None, op0=ALU.mult), [src], [src])
                S.op("pool", lambda e, wv=wv, gg=gg: e.tensor_tensor(out=pooledT.ap[:, gg, :], in0=wv, in1=ext.ap[:, gg, 16:144], op=ALU.subtract), [src, ext], [pooledT])
            pm_ps = PS(3)
            for gg in range(4):
                S.op("pe", lambda e, gg=gg: e.matmul(pm_ps.ap[:, gg * 128:(gg + 1) * 128], pooledT.ap[:, gg, :], wpool.ap[:, gg, :], start=(gg == 0), stop=True, skip_group_check=True), [pooledT, wpool], [pm_ps])
            S.op("dve", lambda e: e.tensor_tensor(out=mixin.ap[:, 512:1024], in0=pm_ps.ap, in1=pscale_bc.ap, op=ALU.mult), [pm_ps, rowb], [mixin])
            pend_pool = S.defer_end()

            S.stage(4)
            if (j + 1) % GB != 0 and j + 1 < NB:
                xnx = acc[(j + 1) % GB]
                S.dma("sp", lambda e, j=j, xnx=xnx: e.dma_start(out=xnx.ap, in_=x_own[(j + 1) * 128:(j + 2) * 128, :]), [], [xnx], x_ctr[(j + 1) % GB])
            nslots = 2 * j + 2
            nk = nslots * 128
            for s in range(nslots):
                sp_ = PS((s % 3) * 2, 2)
                spv = sp_.ap.rearrange("p (h k) -> p h k", h=8)
                rl = relu[s % 3]
                for h in range(8):
                    S.op("pe", lambda e, h=h, s=s, spv=spv: e.matmul(spv[:, h, :], QQT.ap[64:128, h, :], kk.ap[64:128, s * 128:(s + 1) * 128],
                                                                     start=(h % 4 == 0), stop=True, skip_group_check=True), [QQT, kk.bufs[s]], [sp_])
                S.op("act", lambda e, rl=rl, spv=spv: e.activation(out=rl.ap, in_=spv.rearrange("p h k -> p k h"), func=AF.Relu), [sp_], [rl])
                S.op("pool" if s % 3 != 2 else "dve", lambda e, rl=rl: e.tensor_tensor(out=rl.ap, in0=rl.ap, in1=wsc.ap.unsqueeze(1).to_broadcast([128, 128, 8]), op=ALU.mult), [rl, wsc], [rl])
                Ic = It.ap[:, s * 128:(s + 1) * 128]
                Ib = It.bufs[s // 8]
                S.op("dve", lambda e, rl=rl, Ic=Ic: e.tensor_reduce(out=Ic, in_=rl.ap, axis=AX.X, op=ALU.add), [rl], [Ib])
            Ibs = It.bufs[0:(nslots + 7) // 8]
            S.op("dve", lambda e, nk=nk: e.tensor_reduce(out=c_B, in_=It.ap[:, 0:nk], axis=AX.X, op=ALU.max, apply_absolute_value=True), Ibs, [sm])
            S.op("dve", lambda e: e.tensor_scalar(out=It.ap[:, 0:128], in0=It.ap[:, 0:128], scalar1=metat.ap[:, 0:1], scalar2=None, op0=ALU.add), [It.bufs[0], metat], [It.bufs[0]])
            lastI = It.ap[:, (nslots - 1) * 128:nslots * 128]
            S.op("dve", lambda e, lastI=lastI: e.tensor_tensor(out=lastI, in0=lastI, in1=tri.ap, op=ALU.add), [It.bufs[(nslots - 1) // 8], tri], [It.bufs[(nslots - 1) // 8]])
            S.op("pool", lambda e: e.memset(QQTz.ap[64:128, :], 0.0), [], [QQTz])
            S.op("pool", lambda e: e.tensor_copy(out=QQTz.ap[0:64, :], in_=QQT.ap[0:64, :, :].rearrange("p h t -> p (h t)")), [QQT], [QQTz])
            nd_slots = nslots if nslots < 4 else max(1, int(round(nslots * DVE_FRAC)))
            nd = nd_slots * 128
            na = nk - nd
            Ibd = It.bufs[0:(nd_slots + 7) // 8]
            Iba = It.bufs[nd_slots // 8:(nslots + 7) // 8]
            S.op("dve", lambda e: e.tensor_scalar(out=c_B, in0=c_B, scalar1=1.001, scalar2=1e-30, op0=ALU.mult, op1=ALU.add), [sm], [sm])
            S.op("dve", lambda e: e.tensor_scalar(out=steps.ap, in0=pow2_bc.ap, scalar1=c_B, scalar2=None, op0=ALU.mult), [sm, rowb], [steps])
            S.op("dve", lambda e: e.tensor_scalar(out=nsteps.ap, in0=steps.ap, scalar1=-1.0, scalar2=None, op0=ALU.mult), [steps], [nsteps])
            S.op("dve", lambda e: e.tensor_scalar(out=c_lo, in0=c_B, scalar1=-1.0, scalar2=None, op0=ALU.mult), [sm], [sm])
            thr = float(512 - na)
            pend = []
            if j + 1 < NB:
                S.defer_begin()
                other_block(j + 1)
                pend = S.defer_end()
            pend = pend_router_prev + pend_pool + pend
            per_it = (len(pend) + NIT - 1) // NIT
            for it in range(NIT):
                S.emit_some(pend, per_it)
                S.op("dve", lambda e, it=it: e.tensor_tensor(out=tst.ap[:, 0:1], in0=c_lo, in1=steps.ap[:, it:it + 1], op=ALU.add), [sm, steps], [tst])
                S.op("dve", lambda e, nd=nd: e.tensor_scalar(out=c_g.to_broadcast([128, nd]), in0=It.ap[:, 0:nd], scalar1=tst.ap[:, 0:1], scalar2=None,
                                                           op0=ALU.is_ge, op1=ALU.add, accum_out=c_cnt), [tst] + Ibd, [sm])
                if na > 0:
                    S.op("act", lambda e, nd=nd, nk=nk, na=na: e.activation(out=sm2.ap[:, 1:2].to_broadcast([128, na]), in_=It.ap[:, nd:nk], func=AF.Sign,
                                                                          bias=tst.ap[:, 0:1], scale=-1.0, accum_out=sm2.ap[:, 0:1]), [tst] + Iba, [sm2])
                    S.op("dve", lambda e: e.scalar_tensor_tensor(out=c_cnt, in0=c_cnt, scalar=2.0, in1=sm2.ap[:, 0:1], op0=ALU.mult, op1=ALU.subtract), [sm, sm2], [sm])
                    S.op("dve", lambda e, it=it, thr=thr: e.tensor_scalar(out=c_g, in0=c_cnt, scalar1=thr, scalar2=steps.ap[:, it:it + 1], op0=ALU.is_ge, op1=ALU.mult), [sm, steps], [sm])
                else:
                    S.op("dve", lambda e, it=it: e.tensor_scalar(out=c_g, in0=c_cnt, scalar1=256.0, scalar2=steps.ap[:, it:it + 1], op0=ALU.is_ge, op1=ALU.mult), [sm, steps], [sm])
                S.op("dve", lambda e: e.tensor_tensor(out=c_lo, in0=c_lo, in1=c_g, op=ALU.add), [sm], [sm])
            S.emit_some(pend, len(pend))
            if j in (0, 1, 5):
                dump(f"I{j}", T(It.ap[:, 0:nk], Ibs))
                dump(f"lo{j}", sm, ap=c_lo)

            S.stage(5)
            o_ps = PS(4, 2)
            oT = o_ps.ap[0:66, :]

            def emit_m01(s0):
                mbt = Mb[(s0 // 4) % 2]
                ns4 = min(4, nslots - s0)
                S.op("dve", lambda e, mbt=mbt, s0=s0, ns4=ns4: e.tensor_scalar(out=mbt.ap[:, 0:ns4 * 128], in0=It.ap[:, s0 * 128:(s0 + ns4) * 128], scalar1=c_lo, scalar2=None,
                                                                            op0=ALU.is_ge), [sm, It.bufs[s0 // 8]], [mbt])

            def emit_qk(s_):
                lt = PS((s_ % 2) * 2, 2)
                for half in range(2):
                    S.op("pe", lambda e, s_=s_, half=half, lt=lt: e.matmul(lt.ap[:, half * 512:(half + 1) * 512], kk.ap[:, s_ * 128:(s_ + 1) * 128],
                                                                          QQTz.ap[:, half * 512:(half + 1) * 512], start=True, stop=True),
                         [kk.bufs[s_], QQTz], [lt])
                mbt = Mb[(s_ // 4) % 2]
                mt = PS(6 + s_ % 2)
                S.op("pe", lambda e, mbt=mbt, mt=mt, s_=s_: e.transpose(mt.ap.bitcast(BF16)[:, 0:128], mbt.ap[:, (s_ % 4) * 128:(s_ % 4 + 1) * 128], ident_b.ap), [mbt, ident_b], [mt])

            emit_m01(0)
            emit_qk(0)
            for s in range(nslots):
                if s + 1 < nslots:
                    if (s + 1) % 4 == 0:
                        emit_m01(s + 1)
                    emit_qk(s + 1)
                lt = PS((s % 2) * 2, 2)
                mt = PS(6 + s % 2)
                pt = PT[s % len(PT)]
                S.op("act", lambda e, pt=pt, lt=lt: e.activation(out=pt.ap, in_=lt.ap, func=AF.Exp), [lt], [pt])
                S.op("dve", lambda e, pt=pt, mt=mt: e.tensor_tensor(out=pt.ap.rearrange("p (h t) -> p h t", h=8), in0=pt.ap.rearrange("p (h t) -> p h t", h=8),
                                                                   in1=mt.ap.bitcast(BF16)[:, 0:128].unsqueeze(1).to_broadcast([128, 8, 128]), op=ALU.mult), [pt, mt], [pt])
                for half in range(2):
                    S.op("pe", lambda e, s=s, half=half, pt=pt: e.matmul(oT[:, half * 512:(half + 1) * 512], Vt.ap[:, s, :], pt.ap[:, half * 512:(half + 1) * 512],
                                                                        start=(s == 0), stop=(s == nslots - 1)), [pt, Vt.bufs[s]], [o_ps])
            oTs = xn.ap[0:66, :]
            S.op("act", lambda e: e.copy(out=oTs, in_=oT), [o_ps], [xn])
            o2 = PS(0, 2)
            o_v = o2.ap.rearrange("p (h d) -> p h d", h=8)
            for h in range(8):
                S.op("pe", lambda e, h=h: e.transpose(o_v[:, h, 0:66], oTs[:, h * 128:(h + 1) * 128], ident_f.ap[0:66, 0:66]), [xn, ident_f], [o2])
            o_ps = o2
            S.op("dve", lambda e: e.reciprocal(out=c_rden, in_=o_v[:, :, 64]), [o_ps], [sm])
            S.op("dve", lambda e: e.tensor_tensor(out=mixin.ap[:, 0:512].rearrange("p (h d) -> p h d", h=8), in0=o_v[:, :, 0:64],
                                                  in1=c_rden.unsqueeze(2).to_broadcast([128, 8, 64]), op=ALU.mult), [o_ps, sm], [mixin])
            if j in (0, 1, 5):
                dump(f"mixin{j}", mixin)

            S.stage(6)
            defer_router = (g != GB - 1)
            if defer_router:
                S.defer_begin()
            mt = PS(2)
            mtv = mt.ap.bitcast(BF16).rearrange("p (c t) -> p c t", c=8)
            for c in range(8):
                S.op("pe", lambda e, c=c: e.transpose(mtv[:, c, :], mixin.ap[:, c * 128:(c + 1) * 128], ident_b.ap), [mixin, ident_b], [mt])
            S.op("act", lambda e: e.copy(out=hT.ap, in_=mtv), [mt], [hT])
            y1 = PS(6, 2)
            for half in range(2):
                for c in range(8):
                    S.op("pe", lambda e, c=c, half=half: e.matmul(y1.ap[:, half * 512:(half + 1) * 512], hT.ap[:, c, :], wout.ap[:, c, half * 512:(half + 1) * 512],
                                                                  start=(c == 0), stop=(c == 7)), [hT, wout], [y1])
            S.op("dve", lambda e: e.tensor_tensor(out=xn.ap, in0=y1.ap, in1=G1.ap, op=ALU.mult), [y1, G1], [xn])
            S.op("pool", lambda e, xa=xa: e.tensor_tensor(out=xa.ap, in0=xa.ap, in1=xn.ap, op=ALU.add), [xa, xn], [xa])
            if j in (0, 1, 5):
                dump(f"x1_{j}", xa)

            S.stage(7)
            h2b = h2T.bufs[g]
            rms_to_hT(xa, 2, (h2T.ap[:, :, g * 128:(g + 1) * 128], h2b), dst_f32=h2Tf, cs=(CS_OTH if defer_router else None))
            lg_ps = PS(2)
            for c in range(8):
                S.op("pe", lambda e, c=c: e.matmul(lg_ps.ap[:, 0:36], h2Tf.ap[:, c, :], wr.ap[:, c, :], start=(c == 0), stop=(c == 7)), [h2Tf, wr], [lg_ps])
            S.op("dve", lambda e: e.tensor_tensor(out=lg.ap, in0=lg_ps.ap[:, 0:36], in1=brt_bc.ap, op=ALU.add), [lg_ps, rowb], [lg])
            R = rsm.ap
            r_gmax, r_ngmax, r_sumg, r_pg = R[:, 0:1], R[:, 1:2], R[:, 2:3], R[:, 3:4]
            r_ohg, r_eg, r_esel, r_m8 = R[:, 4:8], R[:, 8:12], R[:, 16:24], R[:, 24:32]
            r_d, r_ed, r_tp1, r_tp2 = R[:, 32:33], R[:, 33:34], R[:, 34:35], R[:, 35:36]
            r_w1, r_w2, r_t48 = R[:, 40:48], R[:, 48:56], R[:, 56:88]
            dd = ([lg, rsm], [rsm])
            S.op("dve", lambda e: e.tensor_reduce(out=r_gmax, in_=lg.ap[:, 0:4], axis=AX.X, op=ALU.max), *dd)
            S.op("dve", lambda e: e.tensor_scalar(out=r_ohg, in0=lg.ap[:, 0:4], scalar1=r_gmax, scalar2=None, op0=ALU.is_equal), *dd)
            S.op("dve", lambda e: e.tensor_scalar(out=r_ngmax, in0=r_gmax, scalar1=-1.0, scalar2=None, op0=ALU.mult), *dd)
            S.op("act", lambda e: e.activation(out=r_eg, in_=lg.ap[:, 0:4], func=AF.Exp, bias=r_ngmax, scale=1.0, accum_out=r_sumg), *dd)
            S.op("dve", lambda e: e.reciprocal(out=r_pg, in_=r_sumg), *dd)
            S.op("dve", lambda e: e.tensor_tensor(out=r_t48.rearrange("p (g x) -> p g x", g=4), in0=lg.ap[:, 4:36].rearrange("p (g x) -> p g x", g=4),
                                                  in1=r_ohg.unsqueeze(2).to_broadcast([128, 4, 8]), op=ALU.mult), *dd)
            S.op("dve", lambda e: e.tensor_reduce(out=r_esel, in_=r_t48.rearrange("p (g x) -> p x g", g=4), axis=AX.X, op=ALU.add), *dd)
            S.op("dve", lambda e: e.max(out=r_m8, in_=r_esel), *dd)
            S.op("dve", lambda e: e.tensor_tensor(out=r_d, in0=r_m8[:, 1:2], in1=r_m8[:, 0:1], op=ALU.subtract), *dd)
            S.op("act", lambda e: e.activation(out=r_ed, in_=r_d, func=AF.Exp), *dd)
            S.op("dve", lambda e: e.tensor_scalar(out=r_tp1, in0=r_ed, scalar1=1.0, scalar2=None, op0=ALU.add), *dd)
            S.op("dve", lambda e: e.reciprocal(out=r_tp1, in_=r_tp1), *dd)
            S.op("dve", lambda e: e.tensor_tensor(out=r_tp2, in0=r_ed, in1=r_tp1, op=ALU.mult), *dd)
            S.op("dve", lambda e: e.tensor_scalar(out=r_w1, in0=r_esel, scalar1=r_m8[:, 0:1], scalar2=r_tp1, op0=ALU.is_equal, op1=ALU.mult), *dd)
            S.op("dve", lambda e: e.tensor_scalar(out=r_w2, in0=r_esel, scalar1=r_m8[:, 1:2], scalar2=r_tp2, op0=ALU.is_equal, op1=ALU.mult), *dd)
            S.op("dve", lambda e: e.tensor_tensor(out=r_w1, in0=r_w1, in1=r_w2, op=ALU.add), *dd)
            S.op("dve", lambda e: e.tensor_scalar(out=r_ohg, in0=r_ohg, scalar1=r_pg, scalar2=None, op0=ALU.mult), *dd)
            gt_ = gates[g]
            S.op("dve", lambda e, gt_=gt_: e.tensor_tensor(out=gt_.ap.rearrange("p (g x) -> p g x", g=4), in0=r_ohg.unsqueeze(2).to_broadcast([128, 4, 8]),
                                                          in1=r_w1.unsqueeze(1).to_broadcast([128, 4, 8]), op=ALU.mult), [rsm], [gt_])
            if j in (0, 1, 5):
                dump(f"gates{j}", gt_)
            pend_router = S.defer_end() if defer_router else []
            pend_router_prev = pend_router
            out_blocks.append(j)

            S.stage(8)
            if g == GB - 1:
                S.barrier()
                A.reset(att_end if False else amark)
                accy = [A.tile(f"accy{i}", [D], F32) for i in range(GB)]
                sgh = [A.tile(f"sg{i}", [512], F32) for i in range(2)]
                aTh = [A.tile(f"aT{i}", [512], BF16) for i in range(2)]
                stg_g = A.tile("stg_g", [8, DE], F32)
                stg_u = A.tile("stg_u", [8, DE], F32)
                stg_d = A.tile("stg_d", [2, D], F32)
                wgu = [A.tile(f"wgu{i}", [8, 512], BF16) for i in range(2)]
                wd = [A.tile(f"wd{i}", [2, D], BF16) for i in range(2)]
                outst = A.tile("outst", [D], F32)
                tmpo = A.tile("tmpo", [D], F32)
                if j == GB - 1:
                    moe_ctrs = (S.dctr("wg"), S.dctr("wu"), S.dctr("wd"), [S.dctr("wgu0"), S.dctr("wgu1")], [S.dctr("wdb0"), S.dctr("wdb1")], S.dctr("wscr"))
                wg_ctr, wu_ctr, wd_ctr, wgu_ctr, wdb_ctr, wscr_ctr = moe_ctrs
                for ex in range(NE):
                    wgu_ = wgu[ex % 2]
                    wd_ = wd[ex % 2]
                    if j == GB - 1:
                        S.dma("sp", lambda e, ex=ex: e.dma_start(out=stg_g.ap, in_=w_gate[ex].rearrange("(kc p) f -> p kc f", p=128)), [], [stg_g], wg_ctr)
                        S.dma("sp", lambda e, ex=ex: e.dma_start(out=stg_u.ap, in_=w_up[ex].rearrange("(kc p) f -> p kc f", p=128)), [], [stg_u], wu_ctr)
                        S.dma("sp", lambda e, ex=ex: e.dma_start(out=stg_d.ap, in_=w_down[ex].rearrange("(kc p) n -> p kc n", p=128)), [], [stg_d], wd_ctr)
                        S.op("act", lambda e, wgu_=wgu_: e.copy(out=wgu_.ap[:, :, 0:256], in_=stg_g.ap), [stg_g], [wgu_])
                        S.op("pool", lambda e, wgu_=wgu_: e.tensor_copy(out=wgu_.ap[:, :, 256:512], in_=stg_u.ap), [stg_u], [wgu_])
                        S.op("dve", lambda e, wd_=wd_: e.tensor_copy(out=wd_.ap, in_=stg_d.ap), [stg_d], [wd_])
                        S.dma("sp", lambda e, ex=ex, wgu_=wgu_: e.dma_start(out=wgu_s[ex], in_=wgu_.ap.rearrange("p a b -> p (a b)")), [wgu_], [], wscr_ctr)
                        S.dma("sp", lambda e, ex=ex, wd_=wd_: e.dma_start(out=wd_s[ex], in_=wd_.ap.rearrange("p a b -> p (a b)")), [wd_], [], wscr_ctr)
                    else:
                        S.dma("sp", lambda e, ex=ex, wgu_=wgu_: e.dma_start(out=wgu_.ap.rearrange("p a b -> p (a b)"), in_=wgu_s[ex]), [], [wgu_], wgu_ctr[ex % 2])
                        S.dma("sp", lambda e, ex=ex, wd_=wd_: e.dma_start(out=wd_.ap.rearrange("p a b -> p (a b)"), in_=wd_s[ex]), [], [wd_], wdb_ctr[ex % 2])
                    gu = [PS(b) for b in range(4)]
                    for hh in range(2):
                        for oc in (hh, 2 + hh):
                            for c in range(8):
                                S.op("pe", lambda e, oc=oc, c=c, wgu_=wgu_: e.matmul(gu[oc].ap, wgu_.ap[:, c, oc * 128:(oc + 1) * 128], h2T.ap[:, c, :], start=(c == 0), stop=(c == 7)),
                                     [wgu_, h2T], [gu[oc]])
                        S.op("act", lambda e, hh=hh: e.activation(out=sgh[hh].ap, in_=gu[hh].ap, func=AF.Silu), [gu[hh]], [sgh[hh]])
                        S.op("dve", lambda e, hh=hh: e.tensor_tensor(out=aTh[hh].ap, in0=sgh[hh].ap, in1=gu[2 + hh].ap, op=ALU.mult), [sgh[hh], gu[2 + hh]], [aTh[hh]])
                    for pair in range(GB // 2):
                        blks = (2 * pair, 2 * pair + 1)
                        yps = {blk: PS(4 + (blk % 2) * 2, 2) for blk in blks}
                        for c2 in range(2):
                            for blk in blks:
                                for half in range(2):
                                    S.op("pe", lambda e, half=half, c2=c2, blk=blk, yp=yps[blk], wd_=wd_: e.matmul(yp.ap[:, half * 512:(half + 1) * 512], aTh[c2].ap[:, blk * 128:(blk + 1) * 128],
                                                                                                                wd_.ap[:, c2, half * 512:(half + 1) * 512], start=(c2 == 0), stop=(c2 == 1)),
                                         [aTh[c2], wd_], [yps[blk]])
                        for blk in blks:
                            yp = yps[blk]
                            gcol = gates[blk].ap[:, ex:ex + 1]
                            if ex == 0:
                                S.op("dve", lambda e, blk=blk, yp=yp, gcol=gcol: e.tensor_scalar(out=accy[blk].ap, in0=yp.ap, scalar1=gcol, scalar2=None, op0=ALU.mult), [yp, gates[blk]], [accy[blk]])
                            else:
                                S.op("dve", lambda e, blk=blk, yp=yp, gcol=gcol: e.scalar_tensor_tensor(out=accy[blk].ap, in0=yp.ap, scalar=gcol, in1=accy[blk].ap, op0=ALU.mult, op1=ALU.add),
                                     [yp, gates[blk], accy[blk]], [accy[blk]])
                for blk in range(GB):
                    jb = j - (GB - 1) + blk
                    S.op("dve", lambda e, blk=blk: e.tensor_tensor(out=tmpo.ap, in0=accy[blk].ap, in1=G2.ap, op=ALU.mult), [accy[blk], G2], [tmpo])
                    S.op("dve" if blk % 2 == 0 else "pool", lambda e, blk=blk: e.tensor_tensor(out=outst.ap, in0=tmpo.ap, in1=acc[blk].ap, op=ALU.add), [tmpo, acc[blk]], [outst])
                    S.dma("sp", lambda e, jb=jb: e.dma_start(out=out_d[jb * 128:(jb + 1) * 128, :], in_=outst.ap), [outst], [], out_ctr[blk])
                S.barrier()
                A.reset(att_end)

        S.final_wait("sp")

        block = es.enter_context(nc.Block())

        @block.sync
        def _(e):
            S.replay("sp", e)

        @block.tensor
        def _(e):
            S.replay("pe", e)

        @block.scalar
        def _(e):
            S.replay("act", e)

        @block.vector
        def _(e):
            S.replay("dve", e)

        @block.gpsimd
        def _(e):
            S.replay("pool", e)

    return nc


def _prep_inputs(x, c, positions, w_ada, b_ada, g_norm_mix, g_norm_ffn, w_in, g_q, g_k, g_kidx,
                 w_pool, pool_scale, w_out, w_router_group, b_router_group, w_router_expert,
                 b_router_expert, w_gate, w_up, w_down, cores=range(8)):
    f = np.float32
    x = np.asarray(x, f)
    W = np.asarray(w_in[0], f)
    perm = np.concatenate([np.arange(0, 512), np.arange(640, 1152), np.arange(512, 576), np.arange(1152, 1216),
                           np.arange(576, 640), np.arange(1216, 1224), np.arange(1224, 1736)])
    w_in_p = np.ascontiguousarray(W[:, perm])
    inv_freq = (np.float32(10000.0) ** (-np.arange(0, 64, 2, dtype=np.float32) / np.float32(64))).astype(f)
    pow2 = (2.0 ** -np.arange(24)).astype(f)
    rowp = np.zeros((1, 832), f)
    rowp[0, 0:64] = g_q[0]
    rowp[0, 64:128] = g_k[0]
    rowp[0, 128:192] = g_kidx[0]
    rowp[0, 192:704] = pool_scale[0]
    rowp[0, 704:708] = b_router_group[0]
    rowp[0, 708:740] = b_router_expert[0]
    rowp[0, 740:772] = inv_freq
    rowp[0, 772:796] = pow2
    bg = np.concatenate([b_ada[0][2048:3072], b_ada[0][5120:6144]]).astype(f)[None, :]
    w_r = np.ascontiguousarray(np.concatenate([w_router_group[0], w_router_expert[0]], axis=1).astype(f))
    wp = np.ascontiguousarray(np.asarray(w_pool[0], f).transpose(1, 0, 2).reshape(128, 512))
    shared = {
        "rowp": rowp, "bgate": np.ascontiguousarray(bg), "w_ada": np.ascontiguousarray(w_ada[0], f),
        "w_in": w_in_p, "w_out": np.ascontiguousarray(w_out[0], f), "w_pool": wp, "w_r": w_r,
        "w_gate": np.ascontiguousarray(w_gate[0], f), "w_up": np.ascontiguousarray(w_up[0], f),
        "w_down": np.ascontiguousarray(w_down[0], f),
    }
    in_maps = []
    wins = np.array([2, 4, 8, 16], f)
    for core in cores:
        b, p = core // 2, core % 2
        xb = x[b].reshape(64, 128, D)
        pb = np.asarray(positions[b]).reshape(64, 128).astype(np.int32)
        own = xb[p::2]
        pos_own = pb[p::2]
        if p == 1:
            oth = xb[0::2]
            pos_oth = pb[0::2]
        else:
            oth = np.concatenate([np.zeros((1, 128, D), f), xb[1:63:2]], axis=0)
            pos_oth = np.concatenate([np.zeros((1, 128), np.int32), pb[1:63:2]], axis=0)
        pos = np.zeros((128, 64), np.int32)
        pos[:, 0::2] = pos_oth.T
        pos[:, 1::2] = pos_own.T
        colp = np.zeros((128, 72), f)
        colp[:, 0:8] = np.asarray(c[b], f).reshape(8, 128).T
        colp[:, 8:16] = np.asarray(g_norm_mix[0], f).reshape(8, 128).T
        colp[:, 16:24] = np.asarray(g_norm_ffn[0], f).reshape(8, 128).T
        colp[:, 24:72] = np.asarray(b_ada[0], f).reshape(48, 128).T
        meta = np.zeros((128, 2), f)
        meta[:, 0] = 0.0 if p == 1 else NEG
        meta[:, 1] = float(p)
        t = np.arange(128, dtype=f)
        if p == 0:
            cnt = np.minimum(t[None, :] + 1.0, wins[:, None])
        else:
            cnt = np.broadcast_to(wins[:, None], (4, 128))
        rc = np.broadcast_to((1.0 / cnt).astype(f).reshape(1, 512), (128, 512))
        m = dict(shared)
        m.update({
            "x_own": np.ascontiguousarray(own.reshape(-1, D)),
            "x_oth": np.ascontiguousarray(oth.reshape(-1, D)),
            "pos": pos, "colp": colp, "meta": meta, "rc0": np.ascontiguousarray(rc),
        })
        in_maps.append(m)
    return in_maps


_NC_CACHE = {}


def kernel(**inputs):
    if "nc" not in _NC_CACHE:
        _NC_CACHE["nc"] = build_program()
    nc = _NC_CACHE["nc"]
    in_maps = _prep_inputs(**inputs)
    res = run_bass_kernel_spmd(nc, in_maps, core_ids=list(range(8)))
    out = np.zeros((4, 64, 128, D), np.float32)
    for core in range(8):
        b, p = core // 2, core % 2
        out[b, p::2] = np.asarray(res.results[core]["out"]).reshape(NB, 128, D)
    return out.reshape(4, 8192, D)
```
